# Optimizing a Trainium2 kernel written in Bass

```python
import math
import jax, jax.numpy as jnp
from jax import lax
import numpy as np

D_MODEL = 1024
BATCH = 2
SEQ = 16384
DEPTH = 2

D_MIX = D_MODEL
D_A = D_MIX // 4
H_A = 4
HD_A = D_A // H_A
CHUNK = 128
D_B = D_MIX // 2
H_B = 4
DH_B = D_B // (2 * H_B)
D_C = D_MIX // 4
G_C = 4
GD_C = D_C // G_C
POOL_WINDOWS = (2, 4, 8, 16)
D_IN = 2 * D_A + 3 * D_B + D_C
Q_BLOCK = 128
NUM_BUCKETS = 32
MAX_EXACT = NUM_BUCKETS // 2
MAX_DISTANCE = 128
D_FF = 2816
N_EXPERTS = 8
TOP_K = 2
D_FF_EXPERT = 3584
N_DENSE = (DEPTH + 1) // 2
N_MOE = DEPTH // 2
ALPHA = (2 * DEPTH) ** 0.25
BETA = (8 * DEPTH) ** -0.25
EPS = 1e-5

kernel_name = "hybrid_gmlp_diffattn_pool_moe_deepnorm"

F32 = jnp.float32


def layer_norm(x, g, b):
    xf = x.astype(F32)
    mu = jnp.mean(xf, axis=-1, keepdims=True)
    var = jnp.mean(jnp.square(xf - mu), axis=-1, keepdims=True)
    return ((xf - mu) * lax.rsqrt(var + EPS) * g.astype(F32) + b.astype(F32)).astype(x.dtype)


def rms_norm(x, g):
    xf = x.astype(F32)
    ms = jnp.mean(jnp.square(xf), axis=-1, keepdims=True)
    return (xf * lax.rsqrt(ms + EPS) * g.astype(F32)).astype(x.dtype)


def t5_bucket(dist):
    n = jnp.maximum(dist, 0)
    nf = jnp.maximum(n, 1).astype(F32)
    large = MAX_EXACT + (jnp.log(nf / MAX_EXACT) / math.log(MAX_DISTANCE / MAX_EXACT)
                         * (NUM_BUCKETS - MAX_EXACT)).astype(jnp.int32)
    large = jnp.minimum(large, NUM_BUCKETS - 1)
    return jnp.where(n < MAX_EXACT, n, large)


def gmlp_mixer(z, ws, bs):
    bn, s, _ = z.shape
    z = jax.nn.gelu(z, approximate=False)
    u, v = jnp.split(z, 2, axis=-1)
    v = v.reshape(bn, s // CHUNK, CHUNK, H_A, HD_A)
    vf = v.astype(F32)
    mu = jnp.mean(vf, axis=-1, keepdims=True)
    var = jnp.mean(jnp.square(vf - mu), axis=-1, keepdims=True)
    vn = ((vf - mu) * lax.rsqrt(var + EPS)).astype(z.dtype)
    mask = jnp.tril(jnp.ones((CHUNK, CHUNK), dtype=bool))
    w = jnp.where(mask[None], ws, jnp.zeros_like(ws))
    sg = jnp.einsum('hts,bcshd->bcthd', w, vn) + bs.T[None, None, :, :, None]
    return u * sg.reshape(bn, s, D_A)


def diff_attention(q, k, v, rel_bias, lam, subln_g, lam_init):
    bn, s, _ = q.shape
    q = q.reshape(bn, s, H_B, 2, DH_B).transpose(0, 2, 3, 1, 4)
    k = k.reshape(bn, s, H_B, 2, DH_B).transpose(0, 2, 3, 1, 4)
    v = v.reshape(bn, s, H_B, 2 * DH_B).transpose(0, 2, 1, 3)
    nb = s // Q_BLOCK
    qb = q.reshape(bn, H_B, 2, nb, Q_BLOCK, DH_B).transpose(3, 0, 1, 2, 4, 5)
    kpos = jnp.arange(s)
    scale = DH_B ** -0.5

    def block(args):
        i, qblk = args
        qpos = i * Q_BLOCK + jnp.arange(Q_BLOCK)
        dist = qpos[:, None] - kpos[None, :]
        bias = jnp.transpose(rel_bias[t5_bucket(dist)].astype(F32), (2, 0, 1))
        logits = jnp.einsum('bhmqd,bhmkd->bhmqk', qblk, k).astype(F32) * scale
        logits = logits + bias[None, :, None]
        logits = jnp.where(dist >= 0, logits, -jnp.inf)
        p = jax.nn.softmax(logits, axis=-1)
        a = p[:, :, 0] - lam * p[:, :, 1]
        return jnp.einsum('bhqk,bhkv->bhqv', a.astype(v.dtype), v)

    o = lax.map(block, (jnp.arange(nb), qb))
    o = o.transpose(1, 2, 0, 3, 4).reshape(bn, H_B, s, 2 * DH_B)
    o = rms_norm(o, subln_g) * (1.0 - lam_init)
    return o.transpose(0, 2, 1, 3).reshape(bn, s, D_B)


def pool_mixer(c, w_pool, pool_scale):
    bn, s, _ = c.shape
    cf = c.astype(F32).reshape(bn, s, G_C, GD_C)
    cs = jnp.concatenate([jnp.zeros((bn, 1, G_C, GD_C), F32), jnp.cumsum(cf, axis=1)], axis=1)
    t = jnp.arange(s)[:, None]
    win = jnp.array(POOL_WINDOWS, dtype=jnp.int32)[None, :]
    start = jnp.maximum(t + 1 - win, 0)
    count = (t + 1 - start).astype(F32)
    gidx = jnp.arange(G_C)[None, :]
    window_sum = cs[:, 1:] - cs[:, start, gidx]
    y = (window_sum / count[None, :, :, None] - cf).astype(c.dtype)
    y = jnp.einsum('bsgc,gcd->bsgd', y, w_pool).reshape(bn, s, D_C)
    return y * pool_scale


def swiglu(x, w1, w3, w2):
    return (jax.nn.silu(x @ w1) * (x @ w3)) @ w2


def moe_ffn(x, router_w, w1, w3, w2):
    bn, s, d = x.shape
    xt = x.reshape(bn * s, d)
    logits = (xt @ router_w).astype(F32)
    top_v, top_i = lax.top_k(logits, TOP_K)
    top_w = jax.nn.softmax(top_v, axis=-1)
    gates = jnp.sum(jax.nn.one_hot(top_i, N_EXPERTS, dtype=F32) * top_w[..., None], axis=1)
    y = jnp.zeros_like(xt)
    for e in range(N_EXPERTS):
        y = y + gates[:, e:e + 1].astype(x.dtype) * swiglu(xt, w1[e], w3[e], w2[e])
    return y.reshape(bn, s, d)


def setup_inputs(seed: int = 0) -> dict:
    key = jax.random.key(seed)
    ks = jax.random.split(key, 24)
    nrm = lambda k, shape, sc: jax.random.normal(k, shape, F32) * sc
    return {
        "x": nrm(ks[0], (BATCH, SEQ, D_MODEL), 1.0),
        "w_in": nrm(ks[1], (DEPTH, D_MODEL, D_IN), D_MODEL ** -0.5),
        "w_out": nrm(ks[2], (DEPTH, D_MIX, D_MODEL), BETA * D_MIX ** -0.5),
        "gmlp_ws": nrm(ks[3], (DEPTH, H_A, CHUNK, CHUNK), CHUNK ** -0.5),
        "gmlp_bs": 1.0 + nrm(ks[4], (DEPTH, H_A, CHUNK), 0.02),
        "lam_q1": nrm(ks[5], (DEPTH, DH_B), 0.1),
        "lam_k1": nrm(ks[6], (DEPTH, DH_B), 0.1),
        "lam_q2": nrm(ks[7], (DEPTH, DH_B), 0.1),
        "lam_k2": nrm(ks[8], (DEPTH, DH_B), 0.1),
        "diff_subln_g": 1.0 + nrm(ks[9], (DEPTH, 2 * DH_B), 0.02),
        "rel_bias": nrm(ks[10], (NUM_BUCKETS, H_B), 0.5),
        "pool_w": nrm(ks[11], (DEPTH, G_C, GD_C, GD_C), GD_C ** -0.5),
        "pool_scale": 1.0 + nrm(ks[12], (DEPTH, D_C), 0.1),
        "ln1_g": 1.0 + nrm(ks[13], (DEPTH, D_MODEL), 0.02),
        "ln1_b": nrm(ks[14], (DEPTH, D_MODEL), 0.02),
        "ln2_g": 1.0 + nrm(ks[15], (DEPTH, D_MODEL), 0.02),
        "ln2_b": nrm(ks[16], (DEPTH, D_MODEL), 0.02),
        "ffn_w1": nrm(ks[17], (N_DENSE, D_MODEL, D_FF), D_MODEL ** -0.5),
        "ffn_w3": nrm(ks[18], (N_DENSE, D_MODEL, D_FF), D_MODEL ** -0.5),
        "ffn_w2": nrm(ks[19], (N_DENSE, D_FF, D_MODEL), BETA * D_FF ** -0.5),
        "router_w": nrm(ks[20], (N_MOE, D_MODEL, N_EXPERTS), D_MODEL ** -0.5),
        "moe_w1": nrm(ks[21], (N_MOE, N_EXPERTS, D_MODEL, D_FF_EXPERT), D_MODEL ** -0.5),
        "moe_w3": nrm(ks[22], (N_MOE, N_EXPERTS, D_MODEL, D_FF_EXPERT), D_MODEL ** -0.5),
        "moe_w2": nrm(ks[23], (N_MOE, N_EXPERTS, D_FF_EXPERT, D_MODEL), BETA * D_FF_EXPERT ** -0.5),
    }


def reference(x, w_in, w_out, gmlp_ws, gmlp_bs, lam_q1, lam_k1, lam_q2, lam_k2,
              diff_subln_g, rel_bias, pool_w, pool_scale, ln1_g, ln1_b, ln2_g, ln2_b,
              ffn_w1, ffn_w3, ffn_w2, router_w, moe_w1, moe_w3, moe_w2):
    o_a = 2 * D_A
    o_q = o_a + D_B
    o_k = o_q + D_B
    o_v = o_k + D_B
    for l in range(DEPTH):
        p = x @ w_in[l]
        lam_init = 0.8 - 0.6 * math.exp(-0.3 * l)
        lam = (jnp.exp(jnp.sum(lam_q1[l].astype(F32) * lam_k1[l].astype(F32)))
               - jnp.exp(jnp.sum(lam_q2[l].astype(F32) * lam_k2[l].astype(F32))) + lam_init)
        y_a = gmlp_mixer(p[..., :o_a], gmlp_ws[l], gmlp_bs[l])
        y_b = diff_attention(p[..., o_a:o_q], p[..., o_q:o_k], p[..., o_k:o_v],
                             rel_bias, lam, diff_subln_g[l], lam_init)
        y_c = pool_mixer(p[..., o_v:], pool_w[l], pool_scale[l])
        h = jnp.concatenate([y_a, y_b, y_c], axis=-1) @ w_out[l]
        x = layer_norm(ALPHA * x + h, ln1_g[l], ln1_b[l])
        if l % 2 == 0:
            i = l // 2
            f = swiglu(x, ffn_w1[i], ffn_w3[i], ffn_w2[i])
        else:
            i = l // 2
            f = moe_ffn(x, router_w[i], moe_w1[i], moe_w3[i], moe_w2[i])
        x = layer_norm(ALPHA * x + f, ln2_g[l], ln2_b[l])
    return x
```

```python
import contextlib
import math
import numpy as np
import concourse.bass as bass
import concourse.mybir as mybir
from concourse.bass_utils import run_bass_kernel_spmd

F32 = mybir.dt.float32
BF16 = mybir.dt.bfloat16
AF = mybir.ActivationFunctionType
ALU = mybir.AluOpType
AX = mybir.AxisListType

D = 1024
B = 2
S = 16384
DEPTH = 2
NH = 4
D_IN = 2304
O_A = 512
O_Q = 1024
O_K = 1536
O_V = 2048
D_FF = 2816
NE = 8
RDBG = 0
SAME_ENGINE_FIFO = False
NEC = 8
D_FFE = 3584
ALPHA = (2 * DEPTH) ** 0.25
EPS = 1e-5
NEG = -30000.0
TSH = 4096
NCORES = 8


class Buf:
    __slots__ = ("w", "r", "name")

    def __init__(self, name=""):
        self.w = None
        self.r = {}
        self.name = name


class Ctx:
    NDS = 48

    def __init__(self, nc, es):
        self.nc = nc
        self.eng = {"pe": nc.tensor, "act": nc.scalar, "dve": nc.vector, "pool": nc.gpsimd, "sp": nc.sync}
        self.sem = {}
        for k in self.eng:
            self.sem[("e", k)] = es.enter_context(nc.semaphore("s_" + k))
        self.cnt = {k: 0 for k in self.eng}
        self.known = {k: {} for k in self.eng}
        self.dcnt = [0] * self.NDS
        self.dnext = 0
        self.dq = [0, 0, 0]
        for i in range(self.NDS):
            self.sem[("d", i)] = es.enter_context(nc.semaphore("d%d" % i))
        self.pending = {k: False for k in self.eng}
        self.outbufs = []
        self.sem[("c", 0)] = es.enter_context(nc.semaphore("s_cc"))
        self.ccnt = 0

    def _wait(self, e, deps):
        kn = self.known[e]
        best = {}
        for key, val in deps:
            if key == ("e", "pe") and e == "pe":
                continue
            if SAME_ENGINE_FIFO and key == ("e", e) and e in ("dve", "act"):
                continue
            if kn.get(key, 0) >= val:
                continue
            if best.get(key, 0) < val:
                best[key] = val
        for key, val in best.items():
            self.eng[e].wait_ge(self.sem[key], val)
            kn[key] = val

    @staticmethod
    def _deps(reads, writes):
        deps = []
        for b in reads:
            if b.w is not None:
                deps.append(b.w)
        for b in writes:
            if b.w is not None:
                deps.append(b.w)
            deps.extend(b.r.items())
        return deps

    @staticmethod
    def _commit(tok, reads, writes):
        key, val = tok
        for b in reads:
            if b.r.get(key, 0) < val:
                b.r[key] = val
        for b in writes:
            b.w = tok
            b.r = {}

    def op(self, e, fn, reads=(), writes=(), mark=True):
        self._wait(e, self._deps(reads, writes))
        ins = fn(self.eng[e])
        if mark:
            self.cnt[e] += 1
            ins.then_inc(self.sem[("e", e)], 1)
            tok = (("e", e), self.cnt[e])
            self.pending[e] = False
        else:
            tok = (("e", e), self.cnt[e] + 1)
            self.pending[e] = True
        self._commit(tok, reads, writes)
        return ins

    def dma(self, q, out, in_, reads=(), writes=()):
        qi = ("sp", "act", "pool").index(q)
        per = self.NDS // 3
        idx = qi * per + self.dq[qi]
        self.dq[qi] = (self.dq[qi] + 1) % per
        deps = self._deps(reads, writes)
        if self.dcnt[idx]:
            deps.append((("d", idx), self.dcnt[idx]))
        self._wait(q, deps)
        self.dcnt[idx] += 16
        self.eng[q].dma_start(out=out, in_=in_).then_inc(self.sem[("d", idx)], 16)
        self._commit((("d", idx), self.dcnt[idx]), reads, writes)

    def allreduce(self, in_t, out_t, groups, reads=(), writes=()):
        self._wait("pool", self._deps(reads, writes))
        self.ccnt += 1
        self.nc.gpsimd.collective_compute("AllReduce", ALU.add, replica_groups=groups,
                                          ins=[in_t.ap().opt()], outs=[out_t.ap().opt()]).then_inc(self.sem[("c", 0)])
        self._commit((("c", 0), self.ccnt), reads, writes)

    def barrier(self):
        for e in self.eng:
            assert not self.pending[e], e
        toks = [(("e", k), self.cnt[k]) for k in self.eng if self.cnt[k]]
        toks += [(("d", i), self.dcnt[i]) for i in range(self.NDS) if self.dcnt[i]]
        if self.ccnt:
            toks.append((("c", 0), self.ccnt))
        for e in self.eng:
            self._wait(e, [t for t in toks if t[0] != ("e", e)])

    def finish(self, bufs):
        deps = []
        for b in bufs:
            if b.w is not None:
                deps.append(b.w)
        self._wait("sp", deps)


_UID = [0]


def _sb(es, nc, name, shape, dt):
    _UID[0] += 1
    return es.enter_context(nc.sbuf_tensor("%s_u%d" % (name, _UID[0]), list(shape), dt))


def _ps(es, nc, name, shape, dt):
    _UID[0] += 1
    return es.enter_context(nc.psum_tensor("%s_u%d" % (name, _UID[0]), list(shape), dt))


def attn_phase(ctx, x_d, wqkv_d, bd_d, bn_d, bfar_d, lam_d, subg_d, eye_d, o_d, lam_init, seq=S,
               xg=None, og=None, mask_d=None, on_chunk=None):
    nc = ctx.nc
    NG = seq // 512
    NT = seq // 128
    with contextlib.ExitStack() as es:
        ident = _sb(es, nc, "a_ident", [128, 128], BF16)
        wsb = _sb(es, nc, "a_w", [128, 8, 384], BF16)
        bdc = _sb(es, nc, "a_bdc", [128, 128], F32)
        bnc = _sb(es, nc, "a_bnc", [128, 128], F32)
        bfar = _sb(es, nc, "a_bfar", [128, 1], F32)
        lam_t = _sb(es, nc, "a_lam", [128, 256], F32)
        lam_p = _sb(es, nc, "a_lamp", [128, 128], F32)
        lam_s = _sb(es, nc, "a_lams", [128, 4], F32)
        neglam = _sb(es, nc, "a_neglam", [128, 1], F32)
        gsc = _sb(es, nc, "a_gsc", [128, 128], F32)
        neghalf = _sb(es, nc, "a_nh", [128, 1], F32)
        qT = _sb(es, nc, "a_qT", [128, seq], BF16)
        kT = _sb(es, nc, "a_kT", [128, seq], BF16)
        vaug = _sb(es, nc, "a_v", [128, NT, 130], BF16)
        b_const = Buf("const")
        b_q = [Buf("q%d" % g) for g in range(NG)]
        b_k = [Buf("k%d" % g) for g in range(NG)]
        b_v = [Buf("v%d" % g) for g in range(NG)]
        b_vones = Buf("vones")
        if og is not None:
            maskt = _sb(es, nc, "a_mask", [128, 4], F32)
            ctx.dma("sp", maskt[:], mask_d[:, :], writes=[b_const])

        ctx.dma("pool", ident[:], eye_d[:, :], writes=[b_const])
        ctx.dma("pool", wsb[:], wqkv_d.rearrange("(c p) n -> p c n", p=128), writes=[b_const])
        ctx.dma("sp", bdc[:], bd_d[:, :], writes=[b_const])
        ctx.dma("sp", bnc[:], bn_d[:, :], writes=[b_const])
        ctx.dma("sp", bfar[:], bfar_d[:, :], writes=[b_const])
        ctx.dma("sp", lam_t[:], lam_d[:, :], writes=[b_const])
        ctx.dma("sp", gsc[:], subg_d[:, :], writes=[b_const])
        ctx.op("pool", lambda e: e.memset(vaug[:, :, 128:130], 1.0), writes=[b_vones])
        ctx.op("pool", lambda e: e.memset(neghalf[:], -0.5), writes=[b_const])
        ctx.op("dve", lambda e: e.tensor_scalar(bdc[:], bdc[:], bfar[:, 0:1], None, ALU.subtract),
               reads=[b_const], writes=[b_const])
        ctx.op("dve", lambda e: e.tensor_scalar(bnc[:], bnc[:], bfar[:, 0:1], None, ALU.subtract),
               reads=[b_const], writes=[b_const])
        ctx.op("dve", lambda e: e.tensor_tensor(lam_p[:, 0:64], lam_t[:, 0:64], lam_t[:, 64:128], ALU.mult),
               reads=[b_const], writes=[b_const])
        ctx.op("dve", lambda e: e.tensor_tensor(lam_p[:, 64:128], lam_t[:, 128:192], lam_t[:, 192:256], ALU.mult),
               reads=[b_const], writes=[b_const])
        ctx.op("dve", lambda e: e.tensor_reduce(lam_s[:, 0:2], lam_p[:].rearrange("p (a b) -> p a b", a=2), AX.X, ALU.add),
               reads=[b_const], writes=[b_const])
        ctx.op("act", lambda e: e.activation(lam_s[:, 2:4], lam_s[:, 0:2], AF.Exp), reads=[b_const], writes=[b_const])
        ctx.op("dve", lambda e: e.tensor_tensor(neglam[:], lam_s[:, 3:4], lam_s[:, 2:3], ALU.subtract),
               reads=[b_const], writes=[b_const])
        ctx.op("dve", lambda e: e.tensor_scalar(neglam[:], neglam[:], -float(lam_init), None, ALU.add),
               reads=[b_const], writes=[b_const])
        ctx.op("dve", lambda e: e.tensor_scalar(gsc[:], gsc[:], float(1.0 - lam_init), None, ALU.mult),
               reads=[b_const], writes=[b_const])

        with contextlib.ExitStack() as es2:
            xb = [_sb(es2, nc, "a_xb%d" % i, [128, 4, 1024], BF16) for i in range(2)]
            xT = [_sb(es2, nc, "a_xT%d" % i, [128, 8, 512], BF16) for i in range(2)]
            psT = [_ps(es2, nc, "a_psT%d" % i, [128, 1024], BF16) for i in range(2)]
            psq = _ps(es2, nc, "a_psq", [128, 512], F32)
            psk = _ps(es2, nc, "a_psk", [128, 512], F32)
            psv = _ps(es2, nc, "a_psv", [128, 4, 128], F32)
            b_xb = [Buf(), Buf()]
            b_xT = [Buf(), Buf()]
            b_psT = [Buf(), Buf()]
            b_psq, b_psk, b_psv = Buf(), Buf(), Buf()
            tcount = 0
            for g in range(NG):
                i = g % 2
                if xg is None:
                    ctx.dma("pool", xb[i][:], x_d[g * 512:(g + 1) * 512, :].rearrange("(s p) d -> p s d", p=128),
                            writes=[b_xb[i]])
                else:
                    xt_, xbuf_ = xg[g % 8]
                    jj = g // 8
                    ctx.dma("pool", xb[i][:], xt_.ap()[jj * 512:(jj + 1) * 512, :].rearrange("(s p) d -> p s d", p=128),
                            reads=[xbuf_], writes=[b_xb[i]])
                for s in range(4):
                    pi = tcount % 2
                    tcount += 1
                    for c in range(8):
                        ctx.op("pe", lambda e, s=s, c=c, pi=pi: e.transpose(
                            psT[pi][:, c * 128:(c + 1) * 128], xb[i][:, s, c * 128:(c + 1) * 128], ident[:]),
                            reads=[b_xb[i], b_const], writes=[b_psT[pi]], mark=(c == 7))
                    ev = "act" if s % 2 == 0 else "dve"
                    if ev == "act":
                        ctx.op("act", lambda e, s=s, pi=pi: e.copy(
                            xT[i][:, :, s * 128:(s + 1) * 128], psT[pi][:].rearrange("p (c t) -> p c t", c=8)),
                            reads=[b_psT[pi]], writes=[b_xT[i]])
                    else:
                        ctx.op("dve", lambda e, s=s, pi=pi: e.tensor_copy(
                            xT[i][:, :, s * 128:(s + 1) * 128], psT[pi][:].rearrange("p (c t) -> p c t", c=8)),
                            reads=[b_psT[pi]], writes=[b_xT[i]])
                for c in range(8):
                    ctx.op("pe", lambda e, c=c: e.matmul(psq[:], wsb[:, c, 0:128], xT[i][:, c, :],
                                                         start=(c == 0), stop=(c == 7)),
                           reads=[b_xT[i], b_const], writes=[b_psq], mark=(c == 7))
                ctx.op("act", lambda e: e.activation(qT[:, g * 512:(g + 1) * 512], psq[:], AF.Copy, scale=0.125),
                       reads=[b_psq], writes=[b_q[g]])
                for c in range(8):
                    ctx.op("pe", lambda e, c=c: e.matmul(psk[:], wsb[:, c, 128:256], xT[i][:, c, :],
                                                         start=(c == 0), stop=(c == 7)),
                           reads=[b_xT[i], b_const], writes=[b_psk], mark=(c == 7))
                ctx.op("dve", lambda e: e.tensor_copy(kT[:, g * 512:(g + 1) * 512], psk[:]),
                       reads=[b_psk], writes=[b_k[g]])
                for s in range(4):
                    for c in range(8):
                        ctx.op("pe", lambda e, s=s, c=c: e.matmul(psv[:, s, :], xT[i][:, c, s * 128:(s + 1) * 128],
                                                                  wsb[:, c, 256:384], start=(c == 0), stop=(c == 7)),
                               reads=[b_xT[i], b_const], writes=[b_psv], mark=(s == 3 and c == 7))
                ctx.op("act", lambda e: e.copy(vaug[:, g * 4:(g + 1) * 4, 0:128], psv[:]),
                       reads=[b_psv], writes=[b_v[g]])
        ctx.barrier()

        with contextlib.ExitStack() as es2:
            NSB = 2
            Sps2 = [_ps(es2, nc, "a_S_%d" % i, [128, 2, 512], F32) for i in range(NSB)]
            Ops = _ps(es2, nc, "a_O", [128, 3, 512], F32)
            NPB = 3
            PT2 = [_sb(es2, nc, "a_P_%d" % i, [128, 2, 512], BF16) for i in range(NPB)]
            osb = [_sb(es2, nc, "a_osb%d" % i, [128, 3, 512], F32) for i in range(2)]
            sm = [_sb(es2, nc, "a_sm%d" % i, [128, 16], F32) for i in range(2)]
            t1 = [_sb(es2, nc, "a_t1_%d" % i, [128, 128], F32) for i in range(2)]
            od = [_sb(es2, nc, "a_od_%d" % i, [128, 128], F32) for i in range(2)]
            junk = [_sb(es2, nc, "a_junk_%d" % i, [128, 128], F32) for i in range(2)]
            ofin = [_sb(es2, nc, "a_of_%d" % i, [128, 128], BF16) for i in range(4)]
            b_S = [Buf() for _ in range(NSB)]
            b_P = [Buf() for _ in range(NPB)]
            b_O = Buf()
            b_osb = [Buf(), Buf()]
            b_fin = [Buf(), Buf()]
            b_of = [Buf() for _ in range(4)]
            b_out = Buf()
            if og is not None:
                of4 = [_sb(es2, nc, "a_of4_%d" % i, [128, 4, 128], BF16) for i in range(2)]
                b_of4 = [Buf(), Buf()]

            def oslot(m, j):
                i = m * 4 + j
                return i // 3, (i % 3) * 160

            its = [(qg, kt) for qg in range(NG) for kt in range(4 * qg + 4)]

            def emit_S(n):
                qg, kt = its[n]
                a = kt - 4 * qg
                jlo = max(0, a)
                sb = n % NSB
                for m in range(2):
                    ctx.op("pe", lambda e, m=m: e.matmul(
                        Sps2[sb][:, m, jlo * 128:512], kT[m * 64:(m + 1) * 64, kt * 128:(kt + 1) * 128],
                        qT[m * 64:(m + 1) * 64, qg * 512 + jlo * 128:(qg + 1) * 512], start=True, stop=True),
                        reads=[b_k[kt // 4], b_q[qg]], writes=[b_S[sb]], mark=(m == 1))
                for j, bt in ((a, bdc), (a + 1, bnc)):
                    if 0 <= j <= 3:
                        for m in range(2):
                            ctx.op("dve", lambda e, m=m, j=j, bt=bt: e.tensor_tensor(
                                Sps2[sb][:, m, j * 128:(j + 1) * 128], Sps2[sb][:, m, j * 128:(j + 1) * 128],
                                bt[:], ALU.add), reads=[b_S[sb], b_const], writes=[b_S[sb]])
                pb = n % NPB
                ctx.op("act", lambda e: e.activation(
                    PT2[pb][:, :, jlo * 128:512], Sps2[sb][:, :, jlo * 128:512], AF.Exp, bias=bfar[:, 0:1], scale=1.0),
                    reads=[b_S[sb], b_const], writes=[b_P[pb]])

            def emit_AV(n):
                qg, kt = its[n]
                a = kt - 4 * qg
                jlo = max(0, a)
                pb = n % NPB
                last = None
                for j in range(jlo, 4):
                    for m in range(2):
                        last = (j, m)
                seen_banks = set()
                for j in range(jlo, 4):
                    for m in range(2):
                        bk, off = oslot(m, j)
                        st = (kt == 0) and (bk not in seen_banks)
                        seen_banks.add(bk)
                        ctx.op("pe", lambda e, m=m, j=j, bk=bk, off=off, st=st: e.matmul(
                            Ops[:, bk, off:off + 129], PT2[pb][:, m, j * 128:(j + 1) * 128], vaug[:, kt, 0:129],
                            start=st, stop=(kt == 4 * qg + j), skip_group_check=True),
                            reads=[b_P[pb], b_v[kt // 4], b_vones], writes=[b_O], mark=((j, m) == last))
                if kt == 4 * qg + 3:
                    emit_fin(qg)

            def emit_fin(qg):
                oi = qg % 2
                for bk in range(3):
                    ctx.op("dve", lambda e, bk=bk: e.tensor_copy(osb[oi][:, bk, 0:480], Ops[:, bk, 0:480]),
                           reads=[b_O], writes=[b_osb[oi]])
                for j in range(4):
                    fi = j % 2
                    oj = (qg * 4 + j) % 4
                    bk1, of1 = oslot(0, j)
                    bk2, of2 = oslot(1, j)
                    o1 = osb[oi][:, bk1, of1:of1 + 128]
                    o2 = osb[oi][:, bk2, of2:of2 + 128]
                    z1 = osb[oi][:, bk1, of1 + 128:of1 + 129]
                    z2 = osb[oi][:, bk2, of2 + 128:of2 + 129]
                    smt = sm[fi]
                    R_, W_ = [b_osb[oi], b_const, b_fin[fi]], [b_fin[fi]]
                    ctx.op("dve", lambda e: e.reciprocal(smt[:, 0:1], z1), reads=R_, writes=W_)
                    ctx.op("dve", lambda e: e.reciprocal(smt[:, 1:2], z2), reads=R_, writes=W_)
                    ctx.op("dve", lambda e: e.tensor_tensor(smt[:, 2:3], smt[:, 1:2], neglam[:], ALU.mult), reads=R_, writes=W_)
                    ctx.op("dve", lambda e: e.tensor_scalar(t1[fi][:], o1, smt[:, 0:1], None, ALU.mult), reads=R_, writes=W_)
                    ctx.op("dve", lambda e: e.scalar_tensor_tensor(od[fi][:], o2, smt[:, 2:3], t1[fi][:], ALU.mult, ALU.add),
                           reads=R_, writes=W_)
                    ctx.op("dve", lambda e: e.scalar_tensor_tensor(junk[fi][:], od[fi][:], 1.0, od[fi][:], ALU.mult, ALU.mult,
                                                                   accum_out=smt[:, 3:4]), reads=R_, writes=W_)
                    ctx.op("dve", lambda e: e.tensor_scalar(smt[:, 4:5], smt[:, 3:4], 1.0 / 128.0, EPS, ALU.mult, ALU.add),
                           reads=R_, writes=W_)
                    ctx.op("pool", lambda e: e.tensor_tensor(smt[:, 5:6], smt[:, 4:5], neghalf[:], ALU.pow), reads=R_, writes=W_)
                    ctx.op("dve", lambda e: e.scalar_tensor_tensor(ofin[oj][:], od[fi][:], smt[:, 5:6], gsc[:], ALU.mult, ALU.mult),
                           reads=R_ + [b_of[oj]], writes=W_ + [b_of[oj]])
                    row = (qg * 4 + j) * 128
                    if og is None:
                        ob = Buf()
                        ctx.outbufs.append(ob)
                        ctx.dma("sp", o_d[row:row + 128, :], ofin[oj][:], reads=[b_of[oj]], writes=[ob])
                    else:
                        qt_ = qg * 4 + j
                        o4 = of4[qt_ % 2]
                        for sl_ in range(4):
                            ctx.op("dve", lambda e, sl_=sl_: e.tensor_scalar(o4[:, sl_, :], ofin[oj][:], maskt[:, sl_:sl_ + 1], None,
                                                                           ALU.mult),
                                   reads=[b_of[oj], b_const, b_of4[qt_ % 2]], writes=[b_of4[qt_ % 2]])
                        ot_, obuf_ = og[qt_ // 32]
                        r0 = (qt_ % 32) * 128
                        ctx.dma("sp", ot_.ap().rearrange("(j t) d -> t j d", j=4)[r0:r0 + 128, :, :], o4[:],
                                reads=[b_of4[qt_ % 2]], writes=[obuf_])
                        if on_chunk is not None and qt_ % 32 == 31:
                            on_chunk(qt_ // 32)

            n_it = len(its)
            emit_S(0)
            for n in range(n_it):
                if n + 1 < n_it:
                    emit_S(n + 1)
                emit_AV(n)
            ctx.barrier()
        return


def _t5_bucket(dist):
    n = np.maximum(dist, 0)
    nf = np.maximum(n, 1).astype(np.float32)
    large = 16 + (np.log(nf / np.float32(16)) / np.float32(math.log(8.0)) * np.float32(16)).astype(np.int32)
    large = np.minimum(large, 31)
    return np.where(n < 16, n, large)


def _bias_tiles(rel_bias, h):
    k = np.arange(128)[:, None]
    q = np.arange(128)[None, :]
    bd = rel_bias[_t5_bucket(q - k), h].astype(np.float32)
    bd = np.where(q >= k, bd, np.float32(NEG)).astype(np.float32)
    bn = rel_bias[_t5_bucket(q + 128 - k), h].astype(np.float32)
    bfar = np.full((128, 1), rel_bias[31, h], np.float32)
    return np.ascontiguousarray(bd), np.ascontiguousarray(bn), bfar


def _attn_inputs(inp, l, b, h):
    w_in = inp["w_in"][l]
    wq = w_in[:, O_A + h * 128:O_A + (h + 1) * 128]
    wk = w_in[:, O_Q + h * 128:O_Q + (h + 1) * 128]
    wv = w_in[:, O_K + h * 128:O_K + (h + 1) * 128]
    bd, bn, bfar = _bias_tiles(inp["rel_bias"], h)
    lam = np.concatenate([inp["lam_q1"][l], inp["lam_k1"][l], inp["lam_q2"][l], inp["lam_k2"][l]])
    return {
        "wqkv": np.ascontiguousarray(np.concatenate([wq, wk, wv], axis=1), dtype=np.float32),
        "bd": bd, "bn": bn, "bfar": bfar,
        "lam": np.ascontiguousarray(np.broadcast_to(lam[None, :], (128, 256)), dtype=np.float32),
        "subg": np.ascontiguousarray(np.broadcast_to(inp["diff_subln_g"][l][None, :], (128, 128)), dtype=np.float32),
        "eye": np.eye(128, dtype=np.float32),
    }


def _lam_init(l):
    return 0.8 - 0.6 * math.exp(-0.3 * l)


def build_attn_program(l, seq=S):
    nc = bass.Bass("TRN2", target_bir_lowering=False)
    x_d = nc.dram_tensor("x", [seq, D], F32, kind="ExternalInput").ap()
    wqkv_d = nc.dram_tensor("wqkv", [D, 384], F32, kind="ExternalInput").ap()
    bd_d = nc.dram_tensor("bd", [128, 128], F32, kind="ExternalInput").ap()
    bn_d = nc.dram_tensor("bn", [128, 128], F32, kind="ExternalInput").ap()
    bfar_d = nc.dram_tensor("bfar", [128, 1], F32, kind="ExternalInput").ap()
    lam_d = nc.dram_tensor("lam", [128, 256], F32, kind="ExternalInput").ap()
    subg_d = nc.dram_tensor("subg", [128, 128], F32, kind="ExternalInput").ap()
    eye_d = nc.dram_tensor("eye", [128, 128], F32, kind="ExternalInput").ap()
    o_d = nc.dram_tensor("o", [seq, 128], BF16, kind="ExternalOutput").ap()
    with contextlib.ExitStack() as es:
        ctx = Ctx(nc, es)
        attn_phase(ctx, x_d, wqkv_d, bd_d, bn_d, bfar_d, lam_d, subg_d, eye_d, o_d, _lam_init(l), seq=seq)
        ctx.finish(ctx.outbufs)
    return nc


def _ln_tile(ctx, src, dst, g_t, b_t, st, mv, sm, tmp, R, W, neghalf):
    ctx.op("dve", lambda e: e.bn_stats(st[:, 0, :], src[:, 0:512]), reads=R, writes=W)
    yield
    ctx.op("dve", lambda e: e.bn_stats(st[:, 1, :], src[:, 512:1024]), reads=R, writes=W)
    yield
    ctx.op("dve", lambda e: e.bn_aggr(mv[:], st[:]), reads=R, writes=W)
    yield
    ctx.op("dve", lambda e: e.tensor_scalar(sm[:, 0:1], mv[:, 1:2], EPS, None, ALU.add), reads=R, writes=W)
    yield
    ctx.op("pool", lambda e: e.tensor_tensor(sm[:, 1:2], sm[:, 0:1], neghalf[:], ALU.pow), reads=R, writes=W)
    yield
    ctx.op("dve", lambda e: e.tensor_scalar(tmp[:], src[:], mv[:, 0:1], sm[:, 1:2], ALU.subtract, ALU.mult),
           reads=R, writes=W)
    yield
    ctx.op("dve", lambda e: e.tensor_tensor(tmp[:], tmp[:], g_t[:], ALU.mult), reads=R, writes=W)
    yield
    ctx.op("dve", lambda e: e.tensor_tensor(dst[:], tmp[:], b_t[:], ALU.add), reads=R, writes=W)
    yield


def tok_phase(ctx, P, moe, ntok=TSH, tb=1024):
    nc = ctx.nc
    NBLK = ntok // tb
    TPB = tb // 128
    GPB = tb // 512
    dff = D_FFE if moe else D_FF
    nexp = NEC if moe else 1
    nfc = dff // 128
    groups = []
    f0 = 0
    while f0 < nfc:
        gsz = min(4, nfc - f0)
        groups.append((f0, gsz))
        f0 += gsz
    with contextlib.ExitStack() as es:
        identf = _sb(es, nc, "t_identf", [128, 128], F32)
        identb = _sb(es, nc, "t_identb", [128, 128], BF16)
        neghalf = _sb(es, nc, "t_nh", [128, 1], F32)
        yacc = _sb(es, nc, "t_yacc", [128, TPB, 1024], F32)
        xmT = _sb(es, nc, "t_xmT", [128, 8, tb], BF16)
        gates = _sb(es, nc, "t_gates", [128, TPB, 8], F32)
        cT = [_sb(es, nc, "t_cT%d" % i, [128, 2, 528], F32) for i in range(2)]
        b_const = Buf()
        b_yacc = [Buf() for _ in range(TPB)]
        b_xmT = [Buf() for _ in range(TPB)]
        b_gates = [Buf() for _ in range(TPB)]
        b_cT = [Buf(), Buf()]
        ctx.dma("sp", identf[:], P["eye"][:, :], writes=[b_const])
        ctx.dma("pool", identb[:], P["eye"][:, :], writes=[b_const])
        ctx.op("pool", lambda e: e.memset(neghalf[:], -0.5), writes=[b_const])

        for blk in range(NBLK):
            with contextlib.ExitStack() as es2:
                wac = _sb(es2, nc, "m_wac", [128, 8, 768], BF16)
                wout = _sb(es2, nc, "m_wout", [128, 8, 1024], BF16)
                lng = _sb(es2, nc, "m_lng", [128, 1024], F32)
                lnb = _sb(es2, nc, "m_lnb", [128, 1024], F32)
                wmT = _sb(es2, nc, "m_wmT", [128, 4, 128], F32)
                wmTb = _sb(es2, nc, "m_wmTb", [128, 4, 128], BF16)
                maskT = _sb(es2, nc, "m_maskT", [128, 128], F32)
                gbs = _sb(es2, nc, "m_gbs", [128, 4], F32)
                bd = _sb(es2, nc, "m_bd", [128, 2, 128], F32)
                bdb = _sb(es2, nc, "m_bdb", [128, 2, 128], BF16)
                psc = _sb(es2, nc, "m_psc", [128, 256], F32)
                corr = _sb(es2, nc, "m_corr", [128, 2, 16], F32)
                rw = _sb(es2, nc, "m_rw", [128, 8, 8], F32)
                rwh = _sb(es2, nc, "m_rwh", [128, 8, 8], BF16)
                rwl = _sb(es2, nc, "m_rwl", [128, 8, 8], BF16)
                rwt = _sb(es2, nc, "m_rwt", [128, 8, 8], F32)
                xf = [_sb(es2, nc, "m_xf%d" % i, [128, 1024], F32) for i in range(4)]
                xT = _sb(es2, nc, "m_xT", [128, 8, 512], BF16)
                xh = _sb(es2, nc, "m_xh", [16, 1024], F32)
                xhT = _sb(es2, nc, "m_xhT", [128, 8, 16], BF16)
                sA = _sb(es2, nc, "m_sA", [128, 2, 528], F32)
                sB = _sb(es2, nc, "m_sB", [128, 2, 528], F32)
                ymT = _sb(es2, nc, "m_ymT", [128, 2, 512], BF16)
                mixT = _sb(es2, nc, "m_mixT", [128, 8, 512], BF16)
                ob = [_sb(es2, nc, "m_ob%d" % i, [128, 512], BF16) for i in range(2)]
                TSETS = []
                for _pp in range(2):
                    TSETS.append(dict(
                        z=_sb(es2, nc, "m_z", [128, 512], F32), gst=_sb(es2, nc, "m_gst", [128, 4, 6], F32),
                        gmv=_sb(es2, nc, "m_gmv", [128, 4, 2], F32), gsm=_sb(es2, nc, "m_gsm", [128, 8], F32),
                        vn=_sb(es2, nc, "m_vn", [128, 256], BF16), ya=_sb(es2, nc, "m_ya", [128, 256], BF16),
                        xm=_sb(es2, nc, "m_xm", [128, 1024], F32), xmid=_sb(es2, nc, "m_xmid", [128, 1024], F32),
                        lnt=_sb(es2, nc, "m_lnt", [128, 1024], F32), lst=_sb(es2, nc, "m_lst", [128, 2, 6], F32),
                        lmv=_sb(es2, nc, "m_lmv", [128, 2], F32), lsm=_sb(es2, nc, "m_lsm", [128, 4], F32),
                        xlT=_sb(es2, nc, "m_xlT", [128, 8, 128], BF16), rt=_sb(es2, nc, "m_rt", [128, 64], F32),
                        b_z=Buf(), b_g=Buf(), b_xm=Buf(), b_xmid=Buf(), b_ln=Buf(), b_xmTf=Buf(), b_rt=Buf()))
                tp = [_ps(es2, nc, "m_tp%d" % i, [128, 512], F32) for i in range(2)]
                tpb = _ps(es2, nc, "m_tpb", [128, 1024], BF16)
                mm = [_ps(es2, nc, "m_mm%d" % i, [128, 512], F32) for i in range(2)]
                hh = [_ps(es2, nc, "m_hh%d" % i, [128, 512], F32) for i in range(2)]
                sml = _ps(es2, nc, "m_sml", [128, 512], F32)
                b_w = Buf()
                b_xf = [Buf() for _ in range(4)]
                b_xT, b_sA, b_sB, b_ymT, b_mixT = (Buf() for _ in range(5))
                b_ob = [Buf(), Buf()]
                b_tp = [Buf(), Buf()]
                b_tpb = Buf()
                b_mm = [Buf(), Buf()]
                b_hh = [Buf(), Buf()]
                b_sml = Buf()
                b_xh = Buf()

                ctx.dma("pool", wac[:], P["wac"].rearrange("(c p) n -> p c n", p=128), writes=[b_w])
                ctx.dma("pool", wout[:], P["wout"].rearrange("(c p) n -> p c n", p=128), writes=[b_w])
                ctx.dma("sp", lng[:], P["ln1g"][:, :], writes=[b_w])
                ctx.dma("sp", lnb[:], P["ln1b"][:, :], writes=[b_w])
                ctx.dma("sp", wmT[:], P["gws"].rearrange("h s t -> s h t"), writes=[b_w])
                ctx.dma("sp", maskT[:], P["maskT"][:, :], writes=[b_w])
                ctx.dma("sp", gbs[:], P["gbs"][:, :], writes=[b_w])
                ctx.dma("sp", psc[:], P["psc"][:, :], writes=[b_w])
                ctx.dma("sp", corr[:], P["corr"][:, :, :], writes=[b_w])
                if moe:
                    ctx.dma("sp", rw[:], P["rw"].rearrange("(c p) n -> p c n", p=128), writes=[b_w])
                    ctx.op("dve", lambda e: e.tensor_copy(rwh[:], rw[:]), reads=[b_w], writes=[b_w])
                    ctx.op("dve", lambda e: e.tensor_tensor(rwt[:], rw[:], rwh[:], ALU.subtract), reads=[b_w], writes=[b_w])
                    ctx.op("dve", lambda e: e.tensor_copy(rwl[:], rwt[:]), reads=[b_w], writes=[b_w])
                ctx.op("pool", lambda e: e.memset(bd[:], 0.0), reads=[b_w], writes=[b_w])
                for pr in range(2):
                    for gl in range(2):
                        ctx.dma("sp", bd[gl * 64:(gl + 1) * 64, pr, gl * 64:(gl + 1) * 64], P["poolw"][2 * pr + gl, :, :],
                                reads=[b_w], writes=[b_w])
                for pr in range(2):
                    ctx.op("dve", lambda e, pr=pr: e.tensor_tensor(bdb[:, pr, :], bd[:, pr, :], psc[:, pr * 128:(pr + 1) * 128],
                                                                  ALU.mult), reads=[b_w], writes=[b_w])
                for h in range(4):
                    ctx.op("dve", lambda e, h=h: e.tensor_tensor(wmTb[:, h, :], wmT[:, h, :], maskT[:], ALU.mult),
                           reads=[b_w], writes=[b_w])

                for g in range(GPB):
                    gg = blk * GPB + g
                    ci = gg % 2
                    cc = cT[ci]
                    for s in range(4):
                        tl = gg * 4 + s
                        xi = tl % 4
                        ctx.dma("sp", xf[xi][:], P["x"][tl * 128:(tl + 1) * 128, :], reads=P.get("x_rd", []), writes=[b_xf[xi]])
                        for q4 in range(2):
                            ti = (s * 2 + q4) % 2
                            for c4 in range(4):
                                c = q4 * 4 + c4
                                ctx.op("pe", lambda e, c=c, c4=c4, ti=ti: e.transpose(
                                    tp[ti][:, c4 * 128:(c4 + 1) * 128], xf[xi][:, c * 128:(c + 1) * 128], identf[:]),
                                    reads=[b_xf[xi], b_const], writes=[b_tp[ti]], mark=(c4 == 3))
                            ev = "act" if q4 == 0 else "dve"
                            src = tp[ti][:].rearrange("p (c t) -> p c t", c=4)
                            dst = xT[:, q4 * 4:(q4 + 1) * 4, s * 128:(s + 1) * 128]
                            if ev == "act":
                                ctx.op("act", lambda e: e.copy(dst, src), reads=[b_tp[ti]], writes=[b_xT])
                            else:
                                ctx.op("dve", lambda e: e.tensor_copy(dst, src), reads=[b_tp[ti]], writes=[b_xT])
                    pool_done = [False]

                    def pool_gen():
                        if gg == 0:
                            ctx.dma("sp", xh[:], P["xh"][:, :], reads=P.get("xh_rd", []), writes=[b_xh])
                            yield
                            for q4 in range(2):
                                for c4 in range(4):
                                    c = q4 * 4 + c4
                                    ctx.op("pe", lambda e, c=c, c4=c4: e.transpose(
                                        sml[:, c4 * 16:(c4 + 1) * 16], xh[:, c * 128:(c + 1) * 128], identf[0:16, 0:16]),
                                        reads=[b_xh, b_const], writes=[b_sml], mark=(c4 == 3))
                                ctx.op("dve", lambda e, q4=q4: e.tensor_copy(
                                    xhT[:, q4 * 4:(q4 + 1) * 4, :], sml[:, 0:64].rearrange("p (c t) -> p c t", c=4)),
                                    reads=[b_sml], writes=[b_xh])
                                yield
                            for pr in range(2):
                                for c in range(8):
                                    ctx.op("pe", lambda e, c=c, pr=pr: e.matmul(
                                        sml[:, 64 + pr * 16:64 + (pr + 1) * 16], wac[:, c, 512 + pr * 128:512 + (pr + 1) * 128],
                                        xhT[:, c, :], start=(c == 0), stop=(c == 7), skip_group_check=True),
                                        reads=[b_xh, b_w], writes=[b_sml], mark=(c == 7))
                            ctx.op("dve", lambda e: e.tensor_copy(cc[:, :, 0:16], sml[:, 64:96].rearrange("p (a t) -> p a t", a=2)),
                                   reads=[b_sml], writes=[b_cT[ci]])
                            yield
                        else:
                            pc = cT[1 - ci]
                            ctx.op("pool", lambda e: e.tensor_copy(cc[:, :, 0:16], pc[:, :, 512:528]),
                                   reads=[b_cT[1 - ci]], writes=[b_cT[ci]])
                            yield
                        for pr in range(2):
                            mi = pr
                            for c in range(8):
                                ctx.op("pe", lambda e, c=c, pr=pr: e.matmul(
                                    mm[mi][:], wac[:, c, 512 + pr * 128:512 + (pr + 1) * 128], xT[:, c, :],
                                    start=(c == 0), stop=(c == 7)), reads=[b_xT, b_w], writes=[b_mm[mi]], mark=(c == 7))
                            ctx.op("act", lambda e, pr=pr: e.copy(cc[:, pr, 16:528], mm[mi][:]), reads=[b_mm[mi]], writes=[b_cT[ci]])
                            yield
                        Rp = [b_cT[ci], b_sA, b_sB, b_w]
                        ctx.op("pool", lambda e: e.tensor_tensor(sA[:, :, 1:528], cc[:, :, 1:528], cc[:, :, 0:527], ALU.add),
                               reads=Rp, writes=[b_sA])
                        yield
                        ctx.op("pool", lambda e: e.tensor_tensor(sB[:, :, 3:528], sA[:, :, 3:528], sA[:, :, 1:526], ALU.add),
                               reads=Rp, writes=[b_sB])
                        yield
                        if gg == 0:
                            ctx.op("dve", lambda e: e.tensor_tensor(sA[0:64, 0, 16:32], sA[0:64, 0, 16:32], corr[0:64, 0, :], ALU.mult),
                                   reads=Rp, writes=[b_sA])
                            yield
                        ctx.op("dve", lambda e: e.scalar_tensor_tensor(ymT[0:64, 0, :], sA[0:64, 0, 16:528], 0.5, cc[0:64, 0, 16:528],
                                                                       ALU.mult, ALU.subtract), reads=Rp + [b_ymT], writes=[b_ymT])
                        yield
                        ctx.op("pool", lambda e: e.tensor_tensor(sA[:, :, 7:528], sB[:, :, 7:528], sB[:, :, 3:524], ALU.add),
                               reads=Rp + [b_ymT], writes=[b_sA])
                        yield
                        if gg == 0:
                            ctx.op("dve", lambda e: e.tensor_tensor(sB[64:128, 0, 16:32], sB[64:128, 0, 16:32], corr[64:128, 0, :], ALU.mult),
                                   reads=Rp, writes=[b_sB])
                            yield
                        ctx.op("dve", lambda e: e.scalar_tensor_tensor(ymT[64:128, 0, :], sB[64:128, 0, 16:528], 0.25, cc[64:128, 0, 16:528],
                                                                       ALU.mult, ALU.subtract), reads=Rp + [b_ymT], writes=[b_ymT])
                        yield
                        ctx.op("pool", lambda e: e.tensor_tensor(sB[:, :, 15:528], sA[:, :, 15:528], sA[:, :, 7:520], ALU.add),
                               reads=Rp + [b_ymT], writes=[b_sB])
                        yield
                        if gg == 0:
                            ctx.op("dve", lambda e: e.tensor_tensor(sA[0:64, 1, 16:32], sA[0:64, 1, 16:32], corr[0:64, 1, :], ALU.mult),
                                   reads=Rp, writes=[b_sA])
                            yield
                            ctx.op("dve", lambda e: e.tensor_tensor(sB[64:128, 1, 16:32], sB[64:128, 1, 16:32], corr[64:128, 1, :], ALU.mult),
                                   reads=Rp, writes=[b_sB])
                            yield
                        ctx.op("dve", lambda e: e.scalar_tensor_tensor(ymT[0:64, 1, :], sA[0:64, 1, 16:528], 0.125, cc[0:64, 1, 16:528],
                                                                       ALU.mult, ALU.subtract), reads=Rp + [b_ymT], writes=[b_ymT])
                        yield
                        ctx.op("dve", lambda e: e.scalar_tensor_tensor(ymT[64:128, 1, :], sB[64:128, 1, 16:528], 0.0625, cc[64:128, 1, 16:528],
                                                                       ALU.mult, ALU.subtract), reads=Rp + [b_ymT], writes=[b_ymT])
                        yield
                        for pr in range(2):
                            mi = pr
                            ctx.op("pe", lambda e, pr=pr: e.matmul(mm[mi][:], bdb[:, pr, :], ymT[:, pr, :], start=True, stop=True),
                                   reads=[b_ymT, b_w], writes=[b_mm[mi]])
                            ctx.op("act", lambda e, pr=pr: e.copy(mixT[:, 6 + pr, :], mm[mi][:]), reads=[b_mm[mi]], writes=[b_mixT])
                            yield
                        pool_done[0] = True

                    def tile_gen(s):
                        tl = gg * 4 + s
                        tb_i = tl - blk * TPB
                        _T = TSETS[tl % 2]
                        z, gst, gmv, gsm, vn, ya, xm, xmid = (_T[k_] for k_ in ("z", "gst", "gmv", "gsm", "vn", "ya", "xm", "xmid"))
                        lnt, lst, lmv, lsm, xlT, rt = (_T[k_] for k_ in ("lnt", "lst", "lmv", "lsm", "xlT", "rt"))
                        b_z, b_g, b_xm, b_xmid, b_ln, b_xmTf, b_rt = (_T[k_] for k_ in ("b_z", "b_g", "b_xm", "b_xmid", "b_ln", "b_xmTf", "b_rt"))
                        xi = tl % 4
                        oi = tl % 2
                        ctx.dma("sp", ob[oi][:], P["o"][tl * 128:(tl + 1) * 128, :], reads=([P["o_rd"][tl]] if "o_rd" in P else []), writes=[b_ob[oi]])
                        yield
                        mi_ = tl % 2
                        for c in range(8):
                            ctx.op("pe", lambda e, c=c: e.matmul(mm[mi_][:], xT[:, c, s * 128:(s + 1) * 128], wac[:, c, 0:512],
                                                                 start=(c == 0), stop=(c == 7)),
                                   reads=[b_xT, b_w], writes=[b_mm[mi_]], mark=(c == 7))
                        ctx.op("act", lambda e: e.activation(z[:], mm[mi_][:], AF.Gelu), reads=[b_mm[mi_]], writes=[b_z])
                        yield
                        Rg = [b_z, b_g, b_w]
                        for h in range(4):
                            ctx.op("dve", lambda e, h=h: e.bn_stats(gst[:, h, :], z[:, 256 + h * 64:256 + (h + 1) * 64]),
                                   reads=Rg, writes=[b_g])
                            yield
                        for h in range(4):
                            ctx.op("dve", lambda e, h=h: e.bn_aggr(gmv[:, h, :], gst[:, h, :]), reads=Rg, writes=[b_g])
                            yield
                        ctx.op("dve", lambda e: e.tensor_scalar(gsm[:, 0:4], gmv[:, :, 1], EPS, None, ALU.add), reads=Rg, writes=[b_g])
                        yield
                        for h in range(4):
                            ctx.op("pool", lambda e, h=h: e.tensor_tensor(gsm[:, 4 + h:5 + h], gsm[:, h:h + 1], neghalf[:], ALU.pow),
                                   reads=Rg + [b_const], writes=[b_g])
                            yield
                        for h in range(4):
                            ctx.op("dve", lambda e, h=h: e.tensor_scalar(
                                vn[:, h * 64:(h + 1) * 64], z[:, 256 + h * 64:256 + (h + 1) * 64], gmv[:, h, 0:1], gsm[:, 4 + h:5 + h],
                                ALU.subtract, ALU.mult), reads=Rg, writes=[b_g])
                            yield
                        for h in range(4):
                            ctx.op("pe", lambda e, h=h: e.matmul(sml[:, mi_ * 256 + h * 64:mi_ * 256 + (h + 1) * 64], wmTb[:, h, :], vn[:, h * 64:(h + 1) * 64],
                                                                 start=True, stop=True, skip_group_check=True),
                                   reads=[b_g, b_w], writes=[b_sml], mark=(h == 3))
                        for h in range(4):
                            ctx.op("dve", lambda e, h=h: e.scalar_tensor_tensor(
                                ya[:, h * 64:(h + 1) * 64], sml[:, mi_ * 256 + h * 64:mi_ * 256 + (h + 1) * 64], gbs[:, h:h + 1], z[:, h * 64:(h + 1) * 64],
                                ALU.add, ALU.mult), reads=Rg + [b_sml], writes=[b_g])
                            yield
                        for k2 in range(2):
                            ctx.op("pe", lambda e, k2=k2: e.transpose(tpb[:, k2 * 128:(k2 + 1) * 128], ya[:, k2 * 128:(k2 + 1) * 128],
                                                                      identb[:]), reads=[b_g, b_const], writes=[b_tpb], mark=False)
                        for k4 in range(4):
                            ctx.op("pe", lambda e, k4=k4: e.transpose(tpb[:, (2 + k4) * 128:(3 + k4) * 128],
                                                                      ob[oi][:, k4 * 128:(k4 + 1) * 128], identb[:]),
                                   reads=[b_ob[oi], b_const], writes=[b_tpb], mark=(k4 == 3))
                        ctx.op("dve", lambda e: e.tensor_copy(mixT[:, 0:6, s * 128:(s + 1) * 128],
                                                              tpb[:, 0:768].rearrange("p (c t) -> p c t", c=6)),
                               reads=[b_tpb], writes=[b_mixT])
                        yield
                        while not pool_done[0]:
                            yield
                        for hf in range(2):
                            for c in range(8):
                                ctx.op("pe", lambda e, c=c, hf=hf: e.matmul(hh[hf][:], mixT[:, c, s * 128:(s + 1) * 128],
                                                                            wout[:, c, hf * 512:(hf + 1) * 512],
                                                                            start=(c == 0), stop=(c == 7)),
                                       reads=[b_mixT, b_w], writes=[b_hh[hf]], mark=(c == 7))
                        for hf in range(2):
                            ctx.op("dve", lambda e, hf=hf: e.scalar_tensor_tensor(
                                xm[:, hf * 512:(hf + 1) * 512], xf[xi][:, hf * 512:(hf + 1) * 512], float(ALPHA), hh[hf][:],
                                ALU.mult, ALU.add), reads=[b_xf[xi], b_hh[hf], b_ln], writes=[b_xm])
                        yield
                        Rl = [b_xm, b_ln, b_w, b_const, b_xmid]
                        yield from _ln_tile(ctx, xm, xmid, lng, lnb, lst, lmv, lsm, lnt, Rl, [b_ln, b_xmid], neghalf)
                        if "dbg_z" in P and tl == 0:
                            for nm, t_, bb in (("dbg_z", z, b_z), ("dbg_mix", mixT, b_mixT), ("dbg_xm", xm, b_xm),
                                               ("dbg_xmid", xmid, b_xmid), ("dbg_ya", ya, b_g), ("dbg_xT", xT, b_xT)):
                                ob_ = Buf(); ctx.outbufs.append(ob_)
                                ctx.dma("sp", P[nm], t_[:], reads=[bb], writes=[ob_])
                                yield
                        ctx.op("act", lambda e: e.activation(yacc[:, tb_i, :], xmid[:], AF.Copy, scale=float(ALPHA)),
                               reads=[b_xmid], writes=[b_yacc[tb_i]])
                        yield
                        for q4 in range(2):
                            ti = q4
                            for c4 in range(4):
                                c = q4 * 4 + c4
                                ctx.op("pe", lambda e, c=c, c4=c4: e.transpose(
                                    tp[ti][:, c4 * 128:(c4 + 1) * 128], xmid[:, c * 128:(c + 1) * 128], identf[:]),
                                    reads=[b_xmid, b_const], writes=[b_tp[ti]], mark=(c4 == 3))
                            src = tp[ti][:].rearrange("p (c t) -> p c t", c=4)
                            ctx.op("act", lambda e: e.copy(xmT[:, q4 * 4:(q4 + 1) * 4, tb_i * 128:(tb_i + 1) * 128], src),
                                   reads=[b_tp[ti]], writes=[b_xmT[tb_i]])
                            if moe:
                                ctx.op("dve", lambda e: e.tensor_tensor(
                                    xlT[:, q4 * 4:(q4 + 1) * 4, :], src,
                                    xmT[:, q4 * 4:(q4 + 1) * 4, tb_i * 128:(tb_i + 1) * 128], ALU.subtract),
                                    reads=[b_tp[ti], b_xmT[tb_i]], writes=[b_xmTf])
                                yield
                        if moe:
                            terms = [(xmT, rwh, True), (xmT, rwl, True), (xlT, rwh, False)]
                            nmm = 0
                            for (xa, wb, is_hi) in terms:
                                for c in range(8):
                                    lhs = xa[:, c, tb_i * 128:(tb_i + 1) * 128] if is_hi else xa[:, c, :]
                                    ctx.op("pe", lambda e, c=c, lhs=lhs, wb=wb, nmm=nmm: e.matmul(
                                        mm[mi_][:, 0:8], lhs, wb[:, c, :], start=(nmm == 0), stop=(nmm == 23),
                                        skip_group_check=True),
                                        reads=[b_xmTf, b_xmT[tb_i], b_w], writes=[b_mm[mi_]], mark=(nmm == 23))
                                    nmm += 1
                            Rr = [b_rt, b_mm[mi_]]
                            Wr = [b_rt]
                            lg, eq1, l2, eq2 = rt[:, 0:8], rt[:, 8:16], rt[:, 16:24], rt[:, 24:32]
                            m1, m2, dl, ex, w1, w2 = (rt[:, 32 + i:33 + i] for i in range(6))
                            g1 = rt[:, 40:48]
                            ctx.op("dve", lambda e: e.tensor_copy(lg, mm[mi_][:, 0:8]), reads=Rr, writes=Wr)
                            yield
                            if RDBG == 1:
                                ctx.op("dve", lambda e: e.tensor_copy(gates[:, tb_i, :], lg), reads=Rr, writes=Wr + [b_gates[tb_i]])
                                yield
                                return
                            ctx.op("dve", lambda e: e.tensor_reduce(m1, lg, AX.X, ALU.max), reads=Rr, writes=Wr)
                            yield
                            ctx.op("dve", lambda e: e.tensor_scalar(eq1, lg, m1, None, ALU.is_equal), reads=Rr, writes=Wr)
                            yield
                            ctx.op("dve", lambda e: e.scalar_tensor_tensor(l2, eq1, -1e30, lg, ALU.mult, ALU.add), reads=Rr, writes=Wr)
                            yield
                            ctx.op("dve", lambda e: e.tensor_reduce(m2, l2, AX.X, ALU.max), reads=Rr, writes=Wr)
                            yield
                            ctx.op("dve", lambda e: e.tensor_scalar(eq2, l2, m2, None, ALU.is_equal), reads=Rr, writes=Wr)
                            yield
                            ctx.op("dve", lambda e: e.tensor_tensor(dl, m2, m1, ALU.subtract), reads=Rr, writes=Wr)
                            yield
                            ctx.op("act", lambda e: e.activation(ex, dl, AF.Exp), reads=Rr, writes=Wr)
                            yield
                            ctx.op("dve", lambda e: e.tensor_scalar(w1, ex, 1.0, None, ALU.add), reads=Rr, writes=Wr)
                            yield
                            ctx.op("dve", lambda e: e.reciprocal(w1, w1), reads=Rr, writes=Wr)
                            yield
                            ctx.op("dve", lambda e: e.tensor_tensor(w2, ex, w1, ALU.mult), reads=Rr, writes=Wr)
                            yield
                            ctx.op("dve", lambda e: e.tensor_scalar(g1, eq1, w1, None, ALU.mult), reads=Rr, writes=Wr)
                            yield
                            ctx.op("dve", lambda e: e.scalar_tensor_tensor(gates[:, tb_i, :], eq2, w2, g1, ALU.mult, ALU.add),
                                   reads=Rr, writes=Wr + [b_gates[tb_i]])
                            yield
                    for _pair in ((0, 1), (2, 3)):
                        _gens = [tile_gen(_s) for _s in _pair] + ([pool_gen()] if _pair[0] == 0 else [])
                        while _gens:
                            for _gn in list(_gens):
                                try:
                                    next(_gn)
                                except StopIteration:
                                    _gens.remove(_gn)
            ctx.barrier()

            with contextlib.ExitStack() as es2:
                w1s = [_sb(es2, nc, "f_w1_%d" % i, [128, 8, 512], BF16) for i in range(2)]
                w3s = [_sb(es2, nc, "f_w3_%d" % i, [128, 8, 512], BF16) for i in range(2)]
                w2s = [_sb(es2, nc, "f_w2_%d" % i, [128, 4, 1024], BF16) for i in range(2)]
                gT = [_sb(es2, nc, "f_gT%d" % i, [128, 4, 512], BF16) for i in range(2)]
                sl = [_sb(es2, nc, "f_sl%d" % i, [128, 512], F32) for i in range(2)]
                lng = _sb(es2, nc, "f_lng", [128, 1024], F32)
                lnb = _sb(es2, nc, "f_lnb", [128, 1024], F32)
                lnt = _sb(es2, nc, "f_lnt", [128, 1024], F32)
                xo = [_sb(es2, nc, "f_xo%d" % i, [128, 1024], F32) for i in range(2)]
                lst = _sb(es2, nc, "f_lst", [128, 2, 6], F32)
                lmv = _sb(es2, nc, "f_lmv", [128, 2], F32)
                lsm = _sb(es2, nc, "f_lsm", [128, 4], F32)
                h1p = [_ps(es2, nc, "f_h1_%d" % i, [128, 512], F32) for i in range(2)]
                h3p = [_ps(es2, nc, "f_h3_%d" % i, [128, 512], F32) for i in range(2)]
                yp = [_ps(es2, nc, "f_y_%d" % i, [128, 512], F32) for i in range(2)]
                b_wg = [Buf(), Buf()]
                b_gT = [Buf(), Buf()]
                b_sl = [Buf(), Buf()]
                b_h1 = [Buf(), Buf()]
                b_h3 = [Buf(), Buf()]
                b_yp = [Buf(), Buf()]
                b_lnw, b_ln = Buf(), Buf()
                b_xo = [Buf(), Buf()]
                ctx.dma("sp", lng[:], P["ln2g"][:, :], writes=[b_lnw])
                ctx.dma("sp", lnb[:], P["ln2b"][:, :], writes=[b_lnw])
                if "xg_in" in P:
                    xo4 = [_sb(es2, nc, "f_xo4_%d" % i, [128, 4, 1024], BF16) for i in range(2)]
                    b_xo4 = [Buf(), Buf()]
                    maskt = _sb(es2, nc, "f_mask", [128, 4], F32)
                    ctx.dma("sp", maskt[:], P["mask"][:, :], writes=[b_lnw])

                work = [(e_, f0_, gsz_) for e_ in range(nexp) for (f0_, gsz_) in groups]

                def load_w(n):
                    e_, f0_, gsz_ = work[n]
                    wi = n % 2
                    c0, c1 = f0_ * 128, (f0_ + gsz_) * 128
                    if moe:
                        s1, s3, s2 = P["w1"][e_], P["w3"][e_], P["w2"][e_]
                    else:
                        s1, s3, s2 = P["w1"], P["w3"], P["w2"]
                    ctx.dma("pool", w1s[wi][:, :, 0:gsz_ * 128], s1[:, c0:c1].rearrange("(c p) n -> p c n", p=128),
                            writes=[b_wg[wi]])
                    ctx.dma("pool", w3s[wi][:, :, 0:gsz_ * 128], s3[:, c0:c1].rearrange("(c p) n -> p c n", p=128),
                            writes=[b_wg[wi]])
                    ctx.dma("pool", w2s[wi][:, 0:gsz_, :], s2[c0:c1, :].rearrange("(c p) n -> p c n", p=128),
                            writes=[b_wg[wi]])

                load_w(0)
                hcnt = 0
                ycnt = 0
                gcnt = 0
                for n in range(len(work)):
                    if n + 1 < len(work):
                        load_w(n + 1)
                    e_, f0_, gsz_ = work[n]
                    wi = n % 2
                    for t5 in range(GPB):
                        gi = gcnt % 2
                        gcnt += 1
                        tiles5 = [b_xmT[t5 * 4 + k] for k in range(4)]
                        for fc in range(gsz_):
                            hi = hcnt % 2
                            hcnt += 1
                            for c in range(8):
                                ctx.op("pe", lambda e, c=c: e.matmul(h1p[hi][:], w1s[wi][:, c, fc * 128:(fc + 1) * 128],
                                                                     xmT[:, c, t5 * 512:(t5 + 1) * 512], start=(c == 0), stop=(c == 7)),
                                       reads=[b_wg[wi]] + tiles5, writes=[b_h1[hi]], mark=(c == 7))
                            for c in range(8):
                                ctx.op("pe", lambda e, c=c: e.matmul(h3p[hi][:], w3s[wi][:, c, fc * 128:(fc + 1) * 128],
                                                                     xmT[:, c, t5 * 512:(t5 + 1) * 512], start=(c == 0), stop=(c == 7)),
                                       reads=[b_wg[wi]] + tiles5, writes=[b_h3[hi]], mark=(c == 7))
                            ctx.op("act", lambda e: e.activation(sl[hi][:], h1p[hi][:], AF.Silu), reads=[b_h1[hi]], writes=[b_sl[hi]])
                            ctx.op("dve", lambda e: e.tensor_tensor(gT[gi][:, fc, :], sl[hi][:], h3p[hi][:], ALU.mult),
                                   reads=[b_sl[hi], b_h3[hi]], writes=[b_gT[gi]])
                        for k in range(4):
                            tb_i = t5 * 4 + k
                            for hf in range(2):
                                yi = ycnt % 2
                                ycnt += 1
                                for fc in range(gsz_):
                                    ctx.op("pe", lambda e, fc=fc: e.matmul(yp[yi][:], gT[gi][:, fc, k * 128:(k + 1) * 128],
                                                                           w2s[wi][:, fc, hf * 512:(hf + 1) * 512],
                                                                           start=(fc == 0), stop=(fc == gsz_ - 1)),
                                           reads=[b_gT[gi], b_wg[wi]], writes=[b_yp[yi]], mark=(fc == gsz_ - 1))
                                sc = gates[:, tb_i, e_:e_ + 1] if moe else 1.0
                                rd = [b_yp[yi], b_yacc[tb_i]] + ([b_gates[tb_i]] if moe else [])
                                ctx.op("dve", lambda e: e.scalar_tensor_tensor(
                                    yacc[:, tb_i, hf * 512:(hf + 1) * 512], yp[yi][:], sc, yacc[:, tb_i, hf * 512:(hf + 1) * 512],
                                    ALU.mult, ALU.add), reads=rd, writes=[b_yacc[tb_i]])
                for tb_i in range(TPB):
                    tl = blk * TPB + tb_i
                    oi = tb_i % 2
                    Rl = [b_yacc[tb_i], b_ln, b_lnw, b_const, b_xo[oi]]
                    for _ in _ln_tile(ctx, yacc[:, tb_i, :], xo[oi], lng, lnb, lst, lmv, lsm, lnt, Rl, [b_ln, b_xo[oi]], neghalf):
                        pass
                    if "xg_in" not in P:
                        ob_ = Buf()
                        ctx.outbufs.append(ob_)
                        ctx.dma("sp", P["xo"][tl * 128:(tl + 1) * 128, :], xo[oi][:], reads=[b_xo[oi]], writes=[ob_])
                    else:
                        ctx.dma("sp", P["xo"][tl * 128:(tl + 1) * 128, :], xo[oi][:], reads=[b_xo[oi]], writes=[P["xo_buf"]])
                        x4 = xo4[oi]
                        for sl_ in range(4):
                            eng_ = "act" if sl_ % 2 == 0 else "dve"
                            if eng_ == "act":
                                ctx.op("act", lambda e, sl_=sl_: e.activation(x4[:, sl_, :], xo[oi][:], AF.Copy,
                                                                              scale=maskt[:, sl_:sl_ + 1]),
                                       reads=[b_xo[oi], b_lnw, b_xo4[oi]], writes=[b_xo4[oi]])
                            else:
                                ctx.op("dve", lambda e, sl_=sl_: e.tensor_scalar(x4[:, sl_, :], xo[oi][:], maskt[:, sl_:sl_ + 1],
                                                                                None, ALU.mult),
                                       reads=[b_xo[oi], b_lnw, b_xo4[oi]], writes=[b_xo4[oi]])
                        xt_, xbuf_ = P["xg_in"][tl // 4]
                        r0 = (tl % 4) * 128
                        ctx.dma("sp", xt_.ap().rearrange("(j t) d -> t j d", j=4)[r0:r0 + 128, :, :], x4[:],
                                reads=[b_xo4[oi]], writes=[xbuf_])
                        if "on_chunk" in P and tl % 4 == 3:
                            P["on_chunk"](tl // 4)
            ctx.barrier()


def _tok_consts(r_is_first):
    maskT = (np.arange(128)[:, None] <= np.arange(128)[None, :]).astype(np.float32)
    corr = np.ones((128, 2, 16), np.float32)
    if r_is_first:
        wins = (2, 4, 8, 16)
        t = np.arange(16)
        for g in range(4):
            w = wins[g]
            corr[(g % 2) * 64:(g % 2 + 1) * 64, g // 2, :] = (w / np.minimum(t + 1, w)).astype(np.float32)[None, :]
    return maskT, corr


def _rep(v, n=128):
    return np.ascontiguousarray(np.broadcast_to(np.asarray(v, np.float32)[None, :], (n, v.shape[0])), dtype=np.float32)


def _tok_inputs(inp, l, x_shard, x_halo, o_shard, r_is_first):
    moe = (l % 2 == 1)
    w_in = inp["w_in"][l]
    maskT, corr = _tok_consts(r_is_first)
    m = {}
    if x_shard is not None:
        m.update({"x": np.ascontiguousarray(x_shard, dtype=np.float32),
                  "xh": np.ascontiguousarray(x_halo, dtype=np.float32),
                  "o": np.ascontiguousarray(o_shard)})
    m.update({
        "wac": np.ascontiguousarray(np.concatenate([w_in[:, 0:O_A], w_in[:, O_V:D_IN]], axis=1), dtype=np.float32),
        "wout": np.ascontiguousarray(inp["w_out"][l], dtype=np.float32),
        "gws": np.ascontiguousarray(np.transpose(inp["gmlp_ws"][l], (0, 2, 1)), dtype=np.float32),
        "maskT": maskT,
        "gbs": np.ascontiguousarray(inp["gmlp_bs"][l].T, dtype=np.float32),
        "poolw": np.ascontiguousarray(inp["pool_w"][l], dtype=np.float32),
        "psc": _rep(inp["pool_scale"][l]),
        "corr": corr,
        "ln1g": _rep(inp["ln1_g"][l]), "ln1b": _rep(inp["ln1_b"][l]),
        "ln2g": _rep(inp["ln2_g"][l]), "ln2b": _rep(inp["ln2_b"][l]),
        "eye": np.eye(128, dtype=np.float32),
    })
    i = l // 2
    if moe:
        m["rw"] = np.ascontiguousarray(inp["router_w"][i], dtype=np.float32)
        m["w1"] = inp["moe_w1"][i]
        m["w3"] = inp["moe_w3"][i]
        m["w2"] = inp["moe_w2"][i]
    else:
        m["w1"] = inp["ffn_w1"][i]
        m["w3"] = inp["ffn_w3"][i]
        m["w2"] = inp["ffn_w2"][i]
    return m


def _tok_dram(nc, moe, ntok, dbg=False, sfx="", fused=False):
    def di(name, shape, dt=F32):
        return nc.dram_tensor(name + sfx, list(shape), dt, kind="ExternalInput").ap()
    P = {}
    if not fused:
        P.update({"x": di("x", [ntok, D]), "xh": di("xh", [16, D]), "o": di("o", [ntok, 512], BF16)})
    P.update({
        "wac": di("wac", [D, 768]), "wout": di("wout", [D, D]), "gws": di("gws", [4, 128, 128]),
        "maskT": di("maskT", [128, 128]), "gbs": di("gbs", [128, 4]), "poolw": di("poolw", [4, 64, 64]),
        "psc": di("psc", [128, 256]), "corr": di("corr", [128, 2, 16]),
        "ln1g": di("ln1g", [128, D]), "ln1b": di("ln1b", [128, D]), "ln2g": di("ln2g", [128, D]), "ln2b": di("ln2b", [128, D]),
        "eye": di("eye", [128, 128]),
    })
    if moe:
        P["rw"] = di("rw", [D, NE])
        P["w1"] = di("w1", [NEC, D, D_FFE])
        P["w3"] = di("w3", [NEC, D, D_FFE])
        P["w2"] = di("w2", [NEC, D_FFE, D])
    else:
        P["w1"] = di("w1", [D, D_FF])
        P["w3"] = di("w3", [D, D_FF])
        P["w2"] = di("w2", [D_FF, D])
    if not fused:
        P["xo"] = nc.dram_tensor("xo", [ntok, D], F32, kind="ExternalOutput").ap()
    if dbg:
        def do(name, shape, dt=F32):
            return nc.dram_tensor(name, list(shape), dt, kind="ExternalOutput").ap()
        P["dbg_z"] = do("dbg_z", [128, 512]); P["dbg_mix"] = do("dbg_mix", [128, 8, 512], BF16)
        P["dbg_xm"] = do("dbg_xm", [128, 1024]); P["dbg_xmid"] = do("dbg_xmid", [128, 1024])
        P["dbg_ya"] = do("dbg_ya", [128, 256], BF16); P["dbg_xT"] = do("dbg_xT", [128, 8, 512], BF16)
    return P


def build_tok_program(l, ntok=TSH, tb=1024, dbg=False):
    moe = (l % 2 == 1)
    nc = bass.Bass("TRN2", target_bir_lowering=False)
    P = _tok_dram(nc, moe, ntok, dbg)
    with contextlib.ExitStack() as es:
        ctx = Ctx(nc, es)
        tok_phase(ctx, P, moe, ntok=ntok, tb=tb)
        ctx.finish(ctx.outbufs)
    return nc


_PROG_CACHE = {}


def _get_prog(kind, l):
    key = (kind, l)
    if key not in _PROG_CACHE:
        _PROG_CACHE[key] = build_attn_program(l) if kind == "attn" else build_tok_program(l)
    return _PROG_CACHE[key]


def kernel_unfused(**inputs):
    inp = {k: np.asarray(v) for k, v in inputs.items()}
    x = np.ascontiguousarray(inp["x"], dtype=np.float32)
    cores = list(range(NCORES))
    nsh = S // TSH
    for l in range(DEPTH):
        nc = _get_prog("attn", l)
        in_maps = []
        for c in cores:
            b, h = c // NH, c % NH
            m = _attn_inputs(inp, l, b, h)
            m["x"] = x[b]
            in_maps.append(m)
        res = run_bass_kernel_spmd(nc, in_maps, core_ids=cores)
        o_full = [np.concatenate([np.asarray(res.results[b * NH + h]["o"]) for h in range(NH)], axis=1) for b in range(B)]
        nc = _get_prog("tok", l)
        in_maps = []
        for c in cores:
            b, r = c // nsh, c % nsh
            xs = x[b, r * TSH:(r + 1) * TSH]
            xh = x[b, r * TSH - 16:r * TSH] if r > 0 else np.zeros((16, D), np.float32)
            in_maps.append(_tok_inputs(inp, l, xs, xh, o_full[b][r * TSH:(r + 1) * TSH], r == 0))
        res = run_bass_kernel_spmd(nc, in_maps, core_ids=cores)
        x = np.stack([np.concatenate([np.asarray(res.results[b * nsh + r]["xo"]) for r in range(nsh)], axis=0)
                      for b in range(B)]).astype(np.float32)
    return x


def kernel(**inputs):
    inp = {k: np.asarray(v) for k, v in inputs.items()}
    inp["x"] = np.ascontiguousarray(inp["x"], dtype=np.float32)
    if "fused" not in _PROG_CACHE:
        _PROG_CACHE["fused"] = build_fused_program()
    nc = _PROG_CACHE["fused"]
    cores = list(range(NCORES))
    in_maps = [_fused_inputs(inp, c) for c in cores]
    res = run_bass_kernel_spmd(nc, in_maps, core_ids=cores)
    nsh = S // TSH
    return np.stack([np.concatenate([np.asarray(res.results[b * nsh + r]["xo"]) for r in range(nsh)], axis=0)
                     for b in range(B)]).astype(np.float32)


GROUPS = [[0, 1, 2, 3], [4, 5, 6, 7]]


def o_combine_alloc(es, nc):
    NS = 4
    return dict(
        NS=NS,
        maskt=_sb(es, nc, "c_mask", [128, 4], F32),
        ld=[[_sb(es, nc, "c_ld%d_%d" % (i, k), [128, 512], BF16) for k in range(4)] for i in range(NS)],
        acc=[_sb(es, nc, "c_acc%d" % i, [128, 512], F32) for i in range(2)],
        res=[_sb(es, nc, "c_res%d" % i, [128, 512], BF16) for i in range(NS)],
        b_m=Buf(), b_ld=[Buf() for _ in range(NS)], b_acc=[Buf(), Buf()], b_res=[Buf() for _ in range(NS)], loaded=[False])


def o_combine(ctx, C, og_out, mask_d, o_mine_t, o_bufs):
    NS = C["NS"]
    maskt, ld, acc, res = C["maskt"], C["ld"], C["acc"], C["res"]
    b_m, b_ld, b_acc, b_res = C["b_m"], C["b_ld"], C["b_acc"], C["b_res"]
    if not C["loaded"][0]:
        ctx.dma("sp", maskt[:], mask_d[:, :], writes=[b_m])
        C["loaded"][0] = True
    for tl in range(TSH // 128):
        i = tl % NS
        a2 = tl % 2
        for k in range(4):
            t_, buf_ = og_out[k]
            ctx.dma("sp", ld[i][k][:].rearrange("p (j d) -> p j d", j=4),
                    t_.ap().rearrange("(j t) d -> t j d", j=4)[tl * 128:(tl + 1) * 128, :, :],
                    reads=[buf_], writes=[b_ld[i]])
        ctx.op("dve", lambda e: e.tensor_scalar(acc[a2][:], ld[i][0][:], maskt[:, 0:1], None, ALU.mult),
               reads=[b_ld[i], b_m, b_acc[a2]], writes=[b_acc[a2]])
        for k in range(1, 4):
            dst = res[i] if k == 3 else acc[a2]
            ctx.op("dve", lambda e, k=k, dst=dst: e.scalar_tensor_tensor(dst[:], ld[i][k][:], maskt[:, k:k + 1], acc[a2][:],
                                                                       ALU.mult, ALU.add),
                   reads=[b_ld[i], b_m, b_acc[a2], b_res[i]], writes=[b_res[i] if k == 3 else b_acc[a2]])
        ctx.dma("act", o_mine_t.ap()[tl * 128:(tl + 1) * 128, :], res[i][:], reads=[b_res[i]], writes=[o_bufs[tl]])


def halo_combine(ctx, xg7, mask_d, xh_t, xh_buf):
    nc = ctx.nc
    t_, buf_ = xg7
    with contextlib.ExitStack() as es:
        maskt = _sb(es, nc, "h_mask", [128, 4], F32)
        t3 = _sb(es, nc, "h_t3", [16, 3, 1024], BF16)
        acc = _sb(es, nc, "h_acc", [16, 1024], F32)
        b_m, b_t, b_a = Buf(), Buf(), Buf()
        ctx.dma("sp", maskt[:], mask_d[:, :], writes=[b_m])
        for j in range(1, 4):
            r0 = (j - 1) * 512 + 496
            ctx.dma("sp", t3[:, j - 1, :], t_.ap()[r0:r0 + 16, :], reads=[buf_], writes=[b_t])
        ctx.op("dve", lambda e: e.tensor_scalar(acc[:], t3[:, 0, :], maskt[0:16, 1:2], None, ALU.mult),
               reads=[b_t, b_m], writes=[b_a])
        for j in (2, 3):
            ctx.op("dve", lambda e, j=j: e.scalar_tensor_tensor(acc[:], t3[:, j - 1, :], maskt[0:16, j:j + 1], acc[:],
                                                               ALU.mult, ALU.add), reads=[b_t, b_m, b_a], writes=[b_a])
        ctx.dma("sp", xh_t.ap()[:, :], acc[:], reads=[b_a], writes=[xh_buf])
    ctx.barrier()


def build_fused_program():
    nc = bass.Bass("TRN2", target_bir_lowering=False)

    def di(name, shape, dt=F32):
        return nc.dram_tensor(name, list(shape), dt, kind="ExternalInput").ap()
    xfull = di("xfull", [S, D])
    A = []
    for l in range(DEPTH):
        A.append({"wqkv": di("wqkv_%d" % l, [D, 384]), "lam": di("lam_%d" % l, [128, 256]), "subg": di("subg_%d" % l, [128, 128])})
    bd_d, bn_d, bfar_d = di("bd", [128, 128]), di("bn", [128, 128]), di("bfar", [128, 1])
    eye_d, mask_d = di("eye_a", [128, 128]), di("mask", [128, 4])
    P = [_tok_dram(nc, (l % 2 == 1), TSH, sfx="_%d" % l, fused=True) for l in range(DEPTH)]
    xs_d, xh_d = di("xs", [TSH, D]), di("xh", [16, D])
    xout = nc.dram_tensor("xo", [TSH, D], F32, kind="ExternalOutput").ap()
    og_in = [[(nc.dram_tensor("og_in_%d_%d" % (l, k), [4 * TSH, 128], BF16), Buf()) for k in range(4)] for l in range(DEPTH)]
    og_out = [[(nc.dram_tensor("og_out_%d_%d" % (l, k), [4 * TSH, 128], BF16), Buf()) for k in range(4)] for l in range(DEPTH)]
    xg_in = [(nc.dram_tensor("xg_in_%d" % k, [4 * 512, D], BF16), Buf()) for k in range(8)]
    xg_out = [(nc.dram_tensor("xg_out_%d" % k, [4 * 512, D], BF16), Buf()) for k in range(8)]
    o_mine = [nc.dram_tensor("o_mine_%d" % l, [TSH, 512], BF16) for l in range(DEPTH)]
    x1_loc = nc.dram_tensor("x1_loc", [TSH, D], F32)
    xh_loc = nc.dram_tensor("xh_loc", [16, D], F32)
    with contextlib.ExitStack() as es:
        ctx = Ctx(nc, es)
        x1_buf, xh_buf = Buf(), Buf()
        OC = o_combine_alloc(es, nc)
        for l in range(DEPTH):
            def _og_chunk(k, l=l):
                ctx.allreduce(og_in[l][k][0], og_out[l][k][0], GROUPS, reads=[og_in[l][k][1]], writes=[og_out[l][k][1]])
            attn_phase(ctx, xfull, A[l]["wqkv"], bd_d, bn_d, bfar_d, A[l]["lam"], A[l]["subg"], eye_d, None, _lam_init(l),
                       xg=(xg_out if l > 0 else None), og=og_in[l], mask_d=mask_d, on_chunk=_og_chunk)
            o_bufs = [Buf() for _ in range(TSH // 128)]
            o_combine(ctx, OC, og_out[l], mask_d, o_mine[l], o_bufs)
            Pl = P[l]
            Pl["o"] = o_mine[l].ap()
            Pl["o_rd"] = o_bufs
            Pl["mask"] = mask_d
            if l == 0:
                Pl["x"], Pl["xh"] = xs_d, xh_d
                Pl["xo"] = x1_loc.ap()
                Pl["xo_buf"] = x1_buf
                Pl["xg_in"] = xg_in

                def _xg_chunk(k):
                    ctx.allreduce(xg_in[k][0], xg_out[k][0], GROUPS, reads=[xg_in[k][1]], writes=[xg_out[k][1]])
                Pl["on_chunk"] = _xg_chunk
            else:
                Pl["x"], Pl["x_rd"] = x1_loc.ap(), [x1_buf]
                Pl["xh"], Pl["xh_rd"] = xh_loc.ap(), [xh_buf]
                Pl["xo"] = xout
            tok_phase(ctx, Pl, (l % 2 == 1))
            if l == 0:
                halo_combine(ctx, xg_out[7], mask_d, xh_loc, xh_buf)
        ctx.finish(ctx.outbufs)
    return nc


def _fused_inputs(inp, c):
    b, r = c // 4, c % 4
    x = inp["x"]
    m = {"xfull": x[b], "xs": np.ascontiguousarray(x[b, r * TSH:(r + 1) * TSH]),
         "xh": np.ascontiguousarray(x[b, r * TSH - 16:r * TSH]) if r > 0 else np.zeros((16, D), np.float32)}
    mask = np.zeros((128, 4), np.float32)
    mask[:, r] = 1.0
    m["mask"] = mask
    for l in range(DEPTH):
        a = _attn_inputs(inp, l, b, r)
        m["wqkv_%d" % l], m["lam_%d" % l], m["subg_%d" % l] = a["wqkv"], a["lam"], a["subg"]
        if l == 0:
            m["bd"], m["bn"], m["bfar"], m["eye_a"] = a["bd"], a["bn"], a["bfar"], a["eye"]
        t = _tok_inputs(inp, l, None, None, None, r == 0)
        for k, v in t.items():
            if k in ("x", "xh", "o"):
                continue
            m[k + "_%d" % l] = v
    return m
```

```python
import contextlib
import math
import numpy as np
import concourse.bass as bass
import concourse.mybir as mybir
from concourse.bass_utils import run_bass_kernel_spmd

F32 = mybir.dt.float32
BF16 = mybir.dt.bfloat16
AF = mybir.ActivationFunctionType
ALU = mybir.AluOpType
AX = mybir.AxisListType

D = 1024
B = 2
S = 16384
DEPTH = 2
NH = 4
D_IN = 2304
O_A = 512
O_Q = 1024
O_K = 1536
O_V = 2048
D_FF = 2816
NE = 8
RDBG = 0
SAME_ENGINE_FIFO = False
NEC = 8
D_FFE = 3584
ALPHA = (2 * DEPTH) ** 0.25
EPS = 1e-5
NEG = -30000.0
TSH = 4096
NCORES = 8


class Buf:
    __slots__ = ("w", "r", "name")

    def __init__(self, name=""):
        self.w = None
        self.r = {}
        self.name = name


class Ctx:
    NDS = 48

    def __init__(self, nc, es):
        self.nc = nc
        self.eng = {"pe": nc.tensor, "act": nc.scalar, "dve": nc.vector, "pool": nc.gpsimd, "sp": nc.sync}
        self.sem = {}
        for k in self.eng:
            self.sem[("e", k)] = es.enter_context(nc.semaphore("s_" + k))
        self.cnt = {k: 0 for k in self.eng}
        self.known = {k: {} for k in self.eng}
        self.dcnt = [0] * self.NDS
        self.dnext = 0
        self.dq = [0, 0, 0]
        for i in range(self.NDS):
            self.sem[("d", i)] = es.enter_context(nc.semaphore("d%d" % i))
        self.pending = {k: False for k in self.eng}
        self.outbufs = []
        self.sem[("c", 0)] = es.enter_context(nc.semaphore("s_cc"))
        self.ccnt = 0

    def _wait(self, e, deps):
        kn = self.known[e]
        best = {}
        for key, val in deps:
            if key == ("e", "pe") and e == "pe":
                continue
            if SAME_ENGINE_FIFO and key == ("e", e) and e in ("dve", "act"):
                continue
            if kn.get(key, 0) >= val:
                continue
            if best.get(key, 0) < val:
                best[key] = val
        for key, val in best.items():
            self.eng[e].wait_ge(self.sem[key], val)
            kn[key] = val

    @staticmethod
    def _deps(reads, writes):
        deps = []
        for b in reads:
            if b.w is not None:
                deps.append(b.w)
        for b in writes:
            if b.w is not None:
                deps.append(b.w)
            deps.extend(b.r.items())
        return deps

    @staticmethod
    def _commit(tok, reads, writes):
        key, val = tok
        for b in reads:
            if b.r.get(key, 0) < val:
                b.r[key] = val
        for b in writes:
            b.w = tok
            b.r = {}

    def op(self, e, fn, reads=(), writes=(), mark=True):
        self._wait(e, self._deps(reads, writes))
        ins = fn(self.eng[e])
        if mark:
            self.cnt[e] += 1
            ins.then_inc(self.sem[("e", e)], 1)
            tok = (("e", e), self.cnt[e])
            self.pending[e] = False
        else:
            tok = (("e", e), self.cnt[e] + 1)
            self.pending[e] = True
        self._commit(tok, reads, writes)
        return ins

    def dma(self, q, out, in_, reads=(), writes=()):
        qi = ("sp", "act", "pool").index(q)
        per = self.NDS // 3
        idx = qi * per + self.dq[qi]
        self.dq[qi] = (self.dq[qi] + 1) % per
        deps = self._deps(reads, writes)
        if self.dcnt[idx]:
            deps.append((("d", idx), self.dcnt[idx]))
        self._wait(q, deps)
        self.dcnt[idx] += 16
        self.eng[q].dma_start(out=out, in_=in_).then_inc(self.sem[("d", idx)], 16)
        self._commit((("d", idx), self.dcnt[idx]), reads, writes)

    def allreduce(self, in_t, out_t, groups, reads=(), writes=()):
        self._wait("pool", self._deps(reads, writes))
        self.ccnt += 1
        self.nc.gpsimd.collective_compute("AllReduce", ALU.add, replica_groups=groups,
                                          ins=[in_t.ap().opt()], outs=[out_t.ap().opt()]).then_inc(self.sem[("c", 0)])
        self._commit((("c", 0), self.ccnt), reads, writes)

    def barrier(self):
        for e in self.eng:
            assert not self.pending[e], e
        toks = [(("e", k), self.cnt[k]) for k in self.eng if self.cnt[k]]
        toks += [(("d", i), self.dcnt[i]) for i in range(self.NDS) if self.dcnt[i]]
        if self.ccnt:
            toks.append((("c", 0), self.ccnt))
        for e in self.eng:
            self._wait(e, [t for t in toks if t[0] != ("e", e)])

    def finish(self, bufs):
        deps = []
        for b in bufs:
            if b.w is not None:
                deps.append(b.w)
        self._wait("sp", deps)


_UID = [0]


def _sb(es, nc, name, shape, dt):
    _UID[0] += 1
    return es.enter_context(nc.sbuf_tensor("%s_u%d" % (name, _UID[0]), list(shape), dt))


def _ps(es, nc, name, shape, dt):
    _UID[0] += 1
    return es.enter_context(nc.psum_tensor("%s_u%d" % (name, _UID[0]), list(shape), dt))


def attn_phase(ctx, x_d, wqkv_d, bd_d, bn_d, bfar_d, lam_d, subg_d, eye_d, o_d, lam_init, seq=S,
               xg=None, og=None, mask_d=None, on_chunk=None):
    nc = ctx.nc
    NG = seq // 512
    NT = seq // 128
    with contextlib.ExitStack() as es:
        ident = _sb(es, nc, "a_ident", [128, 128], BF16)
        wsb = _sb(es, nc, "a_w", [128, 8, 384], BF16)
        bdc = _sb(es, nc, "a_bdc", [128, 128], F32)
        bnc = _sb(es, nc, "a_bnc", [128, 128], F32)
        bfar = _sb(es, nc, "a_bfar", [128, 1], F32)
        lam_t = _sb(es, nc, "a_lam", [128, 256], F32)
        lam_p = _sb(es, nc, "a_lamp", [128, 128], F32)
        lam_s = _sb(es, nc, "a_lams", [128, 4], F32)
        neglam = _sb(es, nc, "a_neglam", [128, 1], F32)
        gsc = _sb(es, nc, "a_gsc", [128, 128], F32)
        neghalf = _sb(es, nc, "a_nh", [128, 1], F32)
        qT = _sb(es, nc, "a_qT", [128, seq], BF16)
        kT = _sb(es, nc, "a_kT", [128, seq], BF16)
        vaug = _sb(es, nc, "a_v", [128, NT, 130], BF16)
        b_const = Buf("const")
        b_q = [Buf("q%d" % g) for g in range(NG)]
        b_k = [Buf("k%d" % g) for g in range(NG)]
        b_v = [Buf("v%d" % g) for g in range(NG)]
        b_vones = Buf("vones")
        if og is not None:
            maskt = _sb(es, nc, "a_mask", [128, 4], F32)
            ctx.dma("sp", maskt[:], mask_d[:, :], writes=[b_const])

        ctx.dma("pool", ident[:], eye_d[:, :], writes=[b_const])
        ctx.dma("pool", wsb[:], wqkv_d.rearrange("(c p) n -> p c n", p=128), writes=[b_const])
        ctx.dma("sp", bdc[:], bd_d[:, :], writes=[b_const])
        ctx.dma("sp", bnc[:], bn_d[:, :], writes=[b_const])
        ctx.dma("sp", bfar[:], bfar_d[:, :], writes=[b_const])
        ctx.dma("sp", lam_t[:], lam_d[:, :], writes=[b_const])
        ctx.dma("sp", gsc[:], subg_d[:, :], writes=[b_const])
        ctx.op("pool", lambda e: e.memset(vaug[:, :, 128:130], 1.0), writes=[b_vones])
        ctx.op("pool", lambda e: e.memset(neghalf[:], -0.5), writes=[b_const])
        ctx.op("dve", lambda e: e.tensor_scalar(bdc[:], bdc[:], bfar[:, 0:1], None, ALU.subtract),
               reads=[b_const], writes=[b_const])
        ctx.op("dve", lambda e: e.tensor_scalar(bnc[:], bnc[:], bfar[:, 0:1], None, ALU.subtract),
               reads=[b_const], writes=[b_const])
        ctx.op("dve", lambda e: e.tensor_tensor(lam_p[:, 0:64], lam_t[:, 0:64], lam_t[:, 64:128], ALU.mult),
               reads=[b_const], writes=[b_const])
        ctx.op("dve", lambda e: e.tensor_tensor(lam_p[:, 64:128], lam_t[:, 128:192], lam_t[:, 192:256], ALU.mult),
               reads=[b_const], writes=[b_const])
        ctx.op("dve", lambda e: e.tensor_reduce(lam_s[:, 0:2], lam_p[:].rearrange("p (a b) -> p a b", a=2), AX.X, ALU.add),
               reads=[b_const], writes=[b_const])
        ctx.op("act", lambda e: e.activation(lam_s[:, 2:4], lam_s[:, 0:2], AF.Exp), reads=[b_const], writes=[b_const])
        ctx.op("dve", lambda e: e.tensor_tensor(neglam[:], lam_s[:, 3:4], lam_s[:, 2:3], ALU.subtract),
               reads=[b_const], writes=[b_const])
        ctx.op("dve", lambda e: e.tensor_scalar(neglam[:], neglam[:], -float(lam_init), None, ALU.add),
               reads=[b_const], writes=[b_const])
        ctx.op("dve", lambda e: e.tensor_scalar(gsc[:], gsc[:], float(1.0 - lam_init), None, ALU.mult),
               reads=[b_const], writes=[b_const])

        with contextlib.ExitStack() as es2:
            xb = [_sb(es2, nc, "a_xb%d" % i, [128, 4, 1024], BF16) for i in range(2)]
            xT = [_sb(es2, nc, "a_xT%d" % i, [128, 8, 512], BF16) for i in range(2)]
            psT = [_ps(es2, nc, "a_psT%d" % i, [128, 1024], BF16) for i in range(2)]
            psq = _ps(es2, nc, "a_psq", [128, 512], F32)
            psk = _ps(es2, nc, "a_psk", [128, 512], F32)
            psv = _ps(es2, nc, "a_psv", [128, 4, 128], F32)
            b_xb = [Buf(), Buf()]
            b_xT = [Buf(), Buf()]
            b_psT = [Buf(), Buf()]
            b_psq, b_psk, b_psv = Buf(), Buf(), Buf()
            tcount = 0
            for g in range(NG):
                i = g % 2
                if xg is None:
                    ctx.dma("pool", xb[i][:], x_d[g * 512:(g + 1) * 512, :].rearrange("(s p) d -> p s d", p=128),
                            writes=[b_xb[i]])
                else:
                    xt_, xbuf_ = xg[g % 8]
                    jj = g // 8
                    ctx.dma("pool", xb[i][:], xt_.ap()[jj * 512:(jj + 1) * 512, :].rearrange("(s p) d -> p s d", p=128),
                            reads=[xbuf_], writes=[b_xb[i]])
                for s in range(4):
                    pi = tcount % 2
                    tcount += 1
                    for c in range(8):
                        ctx.op("pe", lambda e, s=s, c=c, pi=pi: e.transpose(
                            psT[pi][:, c * 128:(c + 1) * 128], xb[i][:, s, c * 128:(c + 1) * 128], ident[:]),
                            reads=[b_xb[i], b_const], writes=[b_psT[pi]], mark=(c == 7))
                    ev = "act" if s % 2 == 0 else "dve"
                    if ev == "act":
                        ctx.op("act", lambda e, s=s, pi=pi: e.copy(
                            xT[i][:, :, s * 128:(s + 1) * 128], psT[pi][:].rearrange("p (c t) -> p c t", c=8)),
                            reads=[b_psT[pi]], writes=[b_xT[i]])
                    else:
                        ctx.op("dve", lambda e, s=s, pi=pi: e.tensor_copy(
                            xT[i][:, :, s * 128:(s + 1) * 128], psT[pi][:].rearrange("p (c t) -> p c t", c=8)),
                            reads=[b_psT[pi]], writes=[b_xT[i]])
                for c in range(8):
                    ctx.op("pe", lambda e, c=c: e.matmul(psq[:], wsb[:, c, 0:128], xT[i][:, c, :],
                                                         start=(c == 0), stop=(c == 7)),
                           reads=[b_xT[i], b_const], writes=[b_psq], mark=(c == 7))
                ctx.op("act", lambda e: e.activation(qT[:, g * 512:(g + 1) * 512], psq[:], AF.Copy, scale=0.125),
                       reads=[b_psq], writes=[b_q[g]])
                for c in range(8):
                    ctx.op("pe", lambda e, c=c: e.matmul(psk[:], wsb[:, c, 128:256], xT[i][:, c, :],
                                                         start=(c == 0), stop=(c == 7)),
                           reads=[b_xT[i], b_const], writes=[b_psk], mark=(c == 7))
                ctx.op("dve", lambda e: e.tensor_copy(kT[:, g * 512:(g + 1) * 512], psk[:]),
                       reads=[b_psk], writes=[b_k[g]])
                for s in range(4):
                    for c in range(8):
                        ctx.op("pe", lambda e, s=s, c=c: e.matmul(psv[:, s, :], xT[i][:, c, s * 128:(s + 1) * 128],
                                                                  wsb[:, c, 256:384], start=(c == 0), stop=(c == 7)),
                               reads=[b_xT[i], b_const], writes=[b_psv], mark=(s == 3 and c == 7))
                ctx.op("act", lambda e: e.copy(vaug[:, g * 4:(g + 1) * 4, 0:128], psv[:]),
                       reads=[b_psv], writes=[b_v[g]])
        ctx.barrier()

        with contextlib.ExitStack() as es2:
            NSB = 2
            Sps2 = [_ps(es2, nc, "a_S_%d" % i, [128, 2, 512], F32) for i in range(NSB)]
            Ops = _ps(es2, nc, "a_O", [128, 3, 512], F32)
            NPB = 3
            PT2 = [_sb(es2, nc, "a_P_%d" % i, [128, 2, 512], BF16) for i in range(NPB)]
            osb = [_sb(es2, nc, "a_osb%d" % i, [128, 3, 512], F32) for i in range(2)]
            sm = [_sb(es2, nc, "a_sm%d" % i, [128, 16], F32) for i in range(2)]
            t1 = [_sb(es2, nc, "a_t1_%d" % i, [128, 128], F32) for i in range(2)]
            od = [_sb(es2, nc, "a_od_%d" % i, [128, 128], F32) for i in range(2)]
            junk = [_sb(es2, nc, "a_junk_%d" % i, [128, 128], F32) for i in range(2)]
            ofin = [_sb(es2, nc, "a_of_%d" % i, [128, 128], BF16) for i in range(4)]
            b_S = [Buf() for _ in range(NSB)]
            b_P = [Buf() for _ in range(NPB)]
            b_O = Buf()
            b_osb = [Buf(), Buf()]
            b_fin = [Buf(), Buf()]
            b_of = [Buf() for _ in range(4)]
            b_out = Buf()
            if og is not None:
                of4 = [_sb(es2, nc, "a_of4_%d" % i, [128, 4, 128], BF16) for i in range(2)]
                b_of4 = [Buf(), Buf()]

            def oslot(m, j):
                i = m * 4 + j
                return i // 3, (i % 3) * 160

            its = [(qg, kt) for qg in range(NG) for kt in range(4 * qg + 4)]

            def emit_S(n):
                qg, kt = its[n]
                a = kt - 4 * qg
                jlo = max(0, a)
                sb = n % NSB
                for m in range(2):
                    ctx.op("pe", lambda e, m=m: e.matmul(
                        Sps2[sb][:, m, jlo * 128:512], kT[m * 64:(m + 1) * 64, kt * 128:(kt + 1) * 128],
                        qT[m * 64:(m + 1) * 64, qg * 512 + jlo * 128:(qg + 1) * 512], start=True, stop=True),
                        reads=[b_k[kt // 4], b_q[qg]], writes=[b_S[sb]], mark=(m == 1))
                for j, bt in ((a, bdc), (a + 1, bnc)):
                    if 0 <= j <= 3:
                        for m in range(2):
                            ctx.op("dve", lambda e, m=m, j=j, bt=bt: e.tensor_tensor(
                                Sps2[sb][:, m, j * 128:(j + 1) * 128], Sps2[sb][:, m, j * 128:(j + 1) * 128],
                                bt[:], ALU.add), reads=[b_S[sb], b_const], writes=[b_S[sb]])
                pb = n % NPB
                ctx.op("act", lambda e: e.activation(
                    PT2[pb][:, :, jlo * 128:512], Sps2[sb][:, :, jlo * 128:512], AF.Exp, bias=bfar[:, 0:1], scale=1.0),
                    reads=[b_S[sb], b_const], writes=[b_P[pb]])

            def emit_AV(n):
                qg, kt = its[n]
                a = kt - 4 * qg
                jlo = max(0, a)
                pb = n % NPB
                last = None
                for j in range(jlo, 4):
                    for m in range(2):
                        last = (j, m)
                seen_banks = set()
                for j in range(jlo, 4):
                    for m in range(2):
                        bk, off = oslot(m, j)
                        st = (kt == 0) and (bk not in seen_banks)
                        seen_banks.add(bk)
                        ctx.op("pe", lambda e, m=m, j=j, bk=bk, off=off, st=st: e.matmul(
                            Ops[:, bk, off:off + 129], PT2[pb][:, m, j * 128:(j + 1) * 128], vaug[:, kt, 0:129],
                            start=st, stop=(kt == 4 * qg + j), skip_group_check=True),
                            reads=[b_P[pb], b_v[kt // 4], b_vones], writes=[b_O], mark=((j, m) == last))
                if kt == 4 * qg + 3:
                    emit_fin(qg)

            def emit_fin(qg):
                oi = qg % 2
                for bk in range(3):
                    ctx.op("dve", lambda e, bk=bk: e.tensor_copy(osb[oi][:, bk, 0:480], Ops[:, bk, 0:480]),
                           reads=[b_O], writes=[b_osb[oi]])
                for j in range(4):
                    fi = j % 2
                    oj = (qg * 4 + j) % 4
                    bk1, of1 = oslot(0, j)
                    bk2, of2 = oslot(1, j)
                    o1 = osb[oi][:, bk1, of1:of1 + 128]
                    o2 = osb[oi][:, bk2, of2:of2 + 128]
                    z1 = osb[oi][:, bk1, of1 + 128:of1 + 129]
                    z2 = osb[oi][:, bk2, of2 + 128:of2 + 129]
                    smt = sm[fi]
                    R_, W_ = [b_osb[oi], b_const, b_fin[fi]], [b_fin[fi]]
                    ctx.op("dve", lambda e: e.reciprocal(smt[:, 0:1], z1), reads=R_, writes=W_)
                    ctx.op("dve", lambda e: e.reciprocal(smt[:, 1:2], z2), reads=R_, writes=W_)
                    ctx.op("dve", lambda e: e.tensor_tensor(smt[:, 2:3], smt[:, 1:2], neglam[:], ALU.mult), reads=R_, writes=W_)
                    ctx.op("dve", lambda e: e.tensor_scalar(t1[fi][:], o1, smt[:, 0:1], None, ALU.mult), reads=R_, writes=W_)
                    ctx.op("dve", lambda e: e.scalar_tensor_tensor(od[fi][:], o2, smt[:, 2:3], t1[fi][:], ALU.mult, ALU.add),
                           reads=R_, writes=W_)
                    ctx.op("dve", lambda e: e.scalar_tensor_tensor(junk[fi][:], od[fi][:], 1.0, od[fi][:], ALU.mult, ALU.mult,
                                                                   accum_out=smt[:, 3:4]), reads=R_, writes=W_)
                    ctx.op("dve", lambda e: e.tensor_scalar(smt[:, 4:5], smt[:, 3:4], 1.0 / 128.0, EPS, ALU.mult, ALU.add),
                           reads=R_, writes=W_)
                    ctx.op("pool", lambda e: e.tensor_tensor(smt[:, 5:6], smt[:, 4:5], neghalf[:], ALU.pow), reads=R_, writes=W_)
                    ctx.op("dve", lambda e: e.scalar_tensor_tensor(ofin[oj][:], od[fi][:], smt[:, 5:6], gsc[:], ALU.mult, ALU.mult),
                           reads=R_ + [b_of[oj]], writes=W_ + [b_of[oj]])
                    row = (qg * 4 + j) * 128
                    if og is None:
                        ob = Buf()
                        ctx.outbufs.append(ob)
                        ctx.dma("sp", o_d[row:row + 128, :], ofin[oj][:], reads=[b_of[oj]], writes=[ob])
                    else:
                        qt_ = qg * 4 + j
                        o4 = of4[qt_ % 2]
                        for sl_ in range(4):
                            ctx.op("dve", lambda e, sl_=sl_: e.tensor_scalar(o4[:, sl_, :], ofin[oj][:], maskt[:, sl_:sl_ + 1], None,
                                                                           ALU.mult),
                                   reads=[b_of[oj], b_const, b_of4[qt_ % 2]], writes=[b_of4[qt_ % 2]])
                        ot_, obuf_ = og[qt_ // 32]
                        r0 = (qt_ % 32) * 128
                        ctx.dma("sp", ot_.ap().rearrange("(j t) d -> t j d", j=4)[r0:r0 + 128, :, :], o4[:],
                                reads=[b_of4[qt_ % 2]], writes=[obuf_])
                        if on_chunk is not None and qt_ % 32 == 31:
                            on_chunk(qt_ // 32)

            n_it = len(its)
            emit_S(0)
            for n in range(n_it):
                if n + 1 < n_it:
                    emit_S(n + 1)
                emit_AV(n)
            ctx.barrier()
        return


def _t5_bucket(dist):
    n = np.maximum(dist, 0)
    nf = np.maximum(n, 1).astype(np.float32)
    large = 16 + (np.log(nf / np.float32(16)) / np.float32(math.log(8.0)) * np.float32(16)).astype(np.int32)
    large = np.minimum(large, 31)
    return np.where(n < 16, n, large)


def _bias_tiles(rel_bias, h):
    k = np.arange(128)[:, None]
    q = np.arange(128)[None, :]
    bd = rel_bias[_t5_bucket(q - k), h].astype(np.float32)
    bd = np.where(q >= k, bd, np.float32(NEG)).astype(np.float32)
    bn = rel_bias[_t5_bucket(q + 128 - k), h].astype(np.float32)
    bfar = np.full((128, 1), rel_bias[31, h], np.float32)
    return np.ascontiguousarray(bd), np.ascontiguousarray(bn), bfar


def _attn_inputs(inp, l, b, h):
    w_in = inp["w_in"][l]
    wq = w_in[:, O_A + h * 128:O_A + (h + 1) * 128]
    wk = w_in[:, O_Q + h * 128:O_Q + (h + 1) * 128]
    wv = w_in[:, O_K + h * 128:O_K + (h + 1) * 128]
    bd, bn, bfar = _bias_tiles(inp["rel_bias"], h)
    lam = np.concatenate([inp["lam_q1"][l], inp["lam_k1"][l], inp["lam_q2"][l], inp["lam_k2"][l]])
    return {
        "wqkv": np.ascontiguousarray(np.concatenate([wq, wk, wv], axis=1), dtype=np.float32),
        "bd": bd, "bn": bn, "bfar": bfar,
        "lam": np.ascontiguousarray(np.broadcast_to(lam[None, :], (128, 256)), dtype=np.float32),
        "subg": np.ascontiguousarray(np.broadcast_to(inp["diff_subln_g"][l][None, :], (128, 128)), dtype=np.float32),
        "eye": np.eye(128, dtype=np.float32),
    }


def _lam_init(l):
    return 0.8 - 0.6 * math.exp(-0.3 * l)


def build_attn_program(l, seq=S):
    nc = bass.Bass("TRN2", target_bir_lowering=False)
    x_d = nc.dram_tensor("x", [seq, D], F32, kind="ExternalInput").ap()
    wqkv_d = nc.dram_tensor("wqkv", [D, 384], F32, kind="ExternalInput").ap()
    bd_d = nc.dram_tensor("bd", [128, 128], F32, kind="ExternalInput").ap()
    bn_d = nc.dram_tensor("bn", [128, 128], F32, kind="ExternalInput").ap()
    bfar_d = nc.dram_tensor("bfar", [128, 1], F32, kind="ExternalInput").ap()
    lam_d = nc.dram_tensor("lam", [128, 256], F32, kind="ExternalInput").ap()
    subg_d = nc.dram_tensor("subg", [128, 128], F32, kind="ExternalInput").ap()
    eye_d = nc.dram_tensor("eye", [128, 128], F32, kind="ExternalInput").ap()
    o_d = nc.dram_tensor("o", [seq, 128], BF16, kind="ExternalOutput").ap()
    with contextlib.ExitStack() as es:
        ctx = Ctx(nc, es)
        attn_phase(ctx, x_d, wqkv_d, bd_d, bn_d, bfar_d, lam_d, subg_d, eye_d, o_d, _lam_init(l), seq=seq)
        ctx.finish(ctx.outbufs)
    return nc


def _ln_tile(ctx, src, dst, g_t, b_t, st, mv, sm, tmp, R, W, neghalf):
    ctx.op("dve", lambda e: e.bn_stats(st[:, 0, :], src[:, 0:512]), reads=R, writes=W)
    yield
    ctx.op("dve", lambda e: e.bn_stats(st[:, 1, :], src[:, 512:1024]), reads=R, writes=W)
    yield
    ctx.op("dve", lambda e: e.bn_aggr(mv[:], st[:]), reads=R, writes=W)
    yield
    ctx.op("dve", lambda e: e.tensor_scalar(sm[:, 0:1], mv[:, 1:2], EPS, None, ALU.add), reads=R, writes=W)
    yield
    ctx.op("pool", lambda e: e.tensor_tensor(sm[:, 1:2], sm[:, 0:1], neghalf[:], ALU.pow), reads=R, writes=W)
    yield
    ctx.op("dve", lambda e: e.tensor_scalar(tmp[:], src[:], mv[:, 0:1], sm[:, 1:2], ALU.subtract, ALU.mult),
           reads=R, writes=W)
    yield
    ctx.op("dve", lambda e: e.tensor_tensor(tmp[:], tmp[:], g_t[:], ALU.mult), reads=R, writes=W)
    yield
    ctx.op("dve", lambda e: e.tensor_tensor(dst[:], tmp[:], b_t[:], ALU.add), reads=R, writes=W)
    yield


def tok_phase(ctx, P, moe, ntok=TSH, tb=1024):
    nc = ctx.nc
    NBLK = ntok // tb
    TPB = tb // 128
    GPB = tb // 512
    dff = D_FFE if moe else D_FF
    nexp = NEC if moe else 1
    nfc = dff // 128
    groups = []
    f0 = 0
    while f0 < nfc:
        gsz = min(4, nfc - f0)
        groups.append((f0, gsz))
        f0 += gsz
    with contextlib.ExitStack() as es:
        identf = _sb(es, nc, "t_identf", [128, 128], F32)
        identb = _sb(es, nc, "t_identb", [128, 128], BF16)
        neghalf = _sb(es, nc, "t_nh", [128, 1], F32)
        yacc = _sb(es, nc, "t_yacc", [128, TPB, 1024], F32)
        xmT = _sb(es, nc, "t_xmT", [128, 8, tb], BF16)
        gates = _sb(es, nc, "t_gates", [128, TPB, 8], F32)
        cT = [_sb(es, nc, "t_cT%d" % i, [128, 2, 528], F32) for i in range(2)]
        b_const = Buf()
        b_yacc = [Buf() for _ in range(TPB)]
        b_xmT = [Buf() for _ in range(TPB)]
        b_gates = [Buf() for _ in range(TPB)]
        b_cT = [Buf(), Buf()]
        ctx.dma("sp", identf[:], P["eye"][:, :], writes=[b_const])
        ctx.dma("pool", identb[:], P["eye"][:, :], writes=[b_const])
        ctx.op("pool", lambda e: e.memset(neghalf[:], -0.5), writes=[b_const])

        for blk in range(NBLK):
            with contextlib.ExitStack() as es2:
                wac = _sb(es2, nc, "m_wac", [128, 8, 768], BF16)
                wout = _sb(es2, nc, "m_wout", [128, 8, 1024], BF16)
                lng = _sb(es2, nc, "m_lng", [128, 1024], F32)
                lnb = _sb(es2, nc, "m_lnb", [128, 1024], F32)
                wmT = _sb(es2, nc, "m_wmT", [128, 4, 128], F32)
                wmTb = _sb(es2, nc, "m_wmTb", [128, 4, 128], BF16)
                maskT = _sb(es2, nc, "m_maskT", [128, 128], F32)
                gbs = _sb(es2, nc, "m_gbs", [128, 4], F32)
                bd = _sb(es2, nc, "m_bd", [128, 2, 128], F32)
                bdb = _sb(es2, nc, "m_bdb", [128, 2, 128], BF16)
                psc = _sb(es2, nc, "m_psc", [128, 256], F32)
                corr = _sb(es2, nc, "m_corr", [128, 2, 16], F32)
                rw = _sb(es2, nc, "m_rw", [128, 8, 8], F32)
                rwh = _sb(es2, nc, "m_rwh", [128, 8, 8], BF16)
                rwl = _sb(es2, nc, "m_rwl", [128, 8, 8], BF16)
                rwt = _sb(es2, nc, "m_rwt", [128, 8, 8], F32)
                xf = [_sb(es2, nc, "m_xf%d" % i, [128, 1024], F32) for i in range(4)]
                xT = _sb(es2, nc, "m_xT", [128, 8, 512], BF16)
                xh = _sb(es2, nc, "m_xh", [16, 1024], F32)
                xhT = _sb(es2, nc, "m_xhT", [128, 8, 16], BF16)
                sA = _sb(es2, nc, "m_sA", [128, 2, 528], F32)
                sB = _sb(es2, nc, "m_sB", [128, 2, 528], F32)
                ymT = _sb(es2, nc, "m_ymT", [128, 2, 512], BF16)
                mixT = _sb(es2, nc, "m_mixT", [128, 8, 512], BF16)
                ob = [_sb(es2, nc, "m_ob%d" % i, [128, 512], BF16) for i in range(2)]
                TSETS = []
                for _pp in range(2):
                    TSETS.append(dict(
                        z=_sb(es2, nc, "m_z", [128, 512], F32), gst=_sb(es2, nc, "m_gst", [128, 4, 6], F32),
                        gmv=_sb(es2, nc, "m_gmv", [128, 4, 2], F32), gsm=_sb(es2, nc, "m_gsm", [128, 8], F32),
                        vn=_sb(es2, nc, "m_vn", [128, 256], BF16), ya=_sb(es2, nc, "m_ya", [128, 256], BF16),
                        xm=_sb(es2, nc, "m_xm", [128, 1024], F32), xmid=_sb(es2, nc, "m_xmid", [128, 1024], F32),
                        lnt=_sb(es2, nc, "m_lnt", [128, 1024], F32), lst=_sb(es2, nc, "m_lst", [128, 2, 6], F32),
                        lmv=_sb(es2, nc, "m_lmv", [128, 2], F32), lsm=_sb(es2, nc, "m_lsm", [128, 4], F32),
                        xlT=_sb(es2, nc, "m_xlT", [128, 8, 128], BF16), rt=_sb(es2, nc, "m_rt", [128, 64], F32),
                        b_z=Buf(), b_g=Buf(), b_xm=Buf(), b_xmid=Buf(), b_ln=Buf(), b_xmTf=Buf(), b_rt=Buf()))
                tp = [_ps(es2, nc, "m_tp%d" % i, [128, 512], F32) for i in range(2)]
                tpb = _ps(es2, nc, "m_tpb", [128, 1024], BF16)
                mm = [_ps(es2, nc, "m_mm%d" % i, [128, 512], F32) for i in range(2)]
                hh = [_ps(es2, nc, "m_hh%d" % i, [128, 512], F32) for i in range(2)]
                sml = _ps(es2, nc, "m_sml", [128, 512], F32)
                b_w = Buf()
                b_xf = [Buf() for _ in range(4)]
                b_xT, b_sA, b_sB, b_ymT, b_mixT = (Buf() for _ in range(5))
                b_ob = [Buf(), Buf()]
                b_tp = [Buf(), Buf()]
                b_tpb = Buf()
                b_mm = [Buf(), Buf()]
                b_hh = [Buf(), Buf()]
                b_sml = Buf()
                b_xh = Buf()

                ctx.dma("pool", wac[:], P["wac"].rearrange("(c p) n -> p c n", p=128), writes=[b_w])
                ctx.dma("pool", wout[:], P["wout"].rearrange("(c p) n -> p c n", p=128), writes=[b_w])
                ctx.dma("sp", lng[:], P["ln1g"][:, :], writes=[b_w])
                ctx.dma("sp", lnb[:], P["ln1b"][:, :], writes=[b_w])
                ctx.dma("sp", wmT[:], P["gws"].rearrange("h s t -> s h t"), writes=[b_w])
                ctx.dma("sp", maskT[:], P["maskT"][:, :], writes=[b_w])
                ctx.dma("sp", gbs[:], P["gbs"][:, :], writes=[b_w])
                ctx.dma("sp", psc[:], P["psc"][:, :], writes=[b_w])
                ctx.dma("sp", corr[:], P["corr"][:, :, :], writes=[b_w])
                if moe:
                    ctx.dma("sp", rw[:], P["rw"].rearrange("(c p) n -> p c n", p=128), writes=[b_w])
                    ctx.op("dve", lambda e: e.tensor_copy(rwh[:], rw[:]), reads=[b_w], writes=[b_w])
                    ctx.op("dve", lambda e: e.tensor_tensor(rwt[:], rw[:], rwh[:], ALU.subtract), reads=[b_w], writes=[b_w])
                    ctx.op("dve", lambda e: e.tensor_copy(rwl[:], rwt[:]), reads=[b_w], writes=[b_w])
                ctx.op("pool", lambda e: e.memset(bd[:], 0.0), reads=[b_w], writes=[b_w])
                for pr in range(2):
                    for gl in range(2):
                        ctx.dma("sp", bd[gl * 64:(gl + 1) * 64, pr, gl * 64:(gl + 1) * 64], P["poolw"][2 * pr + gl, :, :],
                                reads=[b_w], writes=[b_w])
                for pr in range(2):
                    ctx.op("dve", lambda e, pr=pr: e.tensor_tensor(bdb[:, pr, :], bd[:, pr, :], psc[:, pr * 128:(pr + 1) * 128],
                                                                  ALU.mult), reads=[b_w], writes=[b_w])
                for h in range(4):
                    ctx.op("dve", lambda e, h=h: e.tensor_tensor(wmTb[:, h, :], wmT[:, h, :], maskT[:], ALU.mult),
                           reads=[b_w], writes=[b_w])

                for g in range(GPB):
                    gg = blk * GPB + g
                    ci = gg % 2
                    cc = cT[ci]
                    for s in range(4):
                        tl = gg * 4 + s
                        xi = tl % 4
                        ctx.dma("sp", xf[xi][:], P["x"][tl * 128:(tl + 1) * 128, :], reads=P.get("x_rd", []), writes=[b_xf[xi]])
                        for q4 in range(2):
                            ti = (s * 2 + q4) % 2
                            for c4 in range(4):
                                c = q4 * 4 + c4
                                ctx.op("pe", lambda e, c=c, c4=c4, ti=ti: e.transpose(
                                    tp[ti][:, c4 * 128:(c4 + 1) * 128], xf[xi][:, c * 128:(c + 1) * 128], identf[:]),
                                    reads=[b_xf[xi], b_const], writes=[b_tp[ti]], mark=(c4 == 3))
                            ev = "act" if q4 == 0 else "dve"
                            src = tp[ti][:].rearrange("p (c t) -> p c t", c=4)
                            dst = xT[:, q4 * 4:(q4 + 1) * 4, s * 128:(s + 1) * 128]
                            if ev == "act":
                                ctx.op("act", lambda e: e.copy(dst, src), reads=[b_tp[ti]], writes=[b_xT])
                            else:
                                ctx.op("dve", lambda e: e.tensor_copy(dst, src), reads=[b_tp[ti]], writes=[b_xT])
                    pool_done = [False]

                    def pool_gen():
                        if gg == 0:
                            ctx.dma("sp", xh[:], P["xh"][:, :], reads=P.get("xh_rd", []), writes=[b_xh])
                            yield
                            for q4 in range(2):
                                for c4 in range(4):
                                    c = q4 * 4 + c4
                                    ctx.op("pe", lambda e, c=c, c4=c4: e.transpose(
                                        sml[:, c4 * 16:(c4 + 1) * 16], xh[:, c * 128:(c + 1) * 128], identf[0:16, 0:16]),
                                        reads=[b_xh, b_const], writes=[b_sml], mark=(c4 == 3))
                                ctx.op("dve", lambda e, q4=q4: e.tensor_copy(
                                    xhT[:, q4 * 4:(q4 + 1) * 4, :], sml[:, 0:64].rearrange("p (c t) -> p c t", c=4)),
                                    reads=[b_sml], writes=[b_xh])
                                yield
                            for pr in range(2):
                                for c in range(8):
                                    ctx.op("pe", lambda e, c=c, pr=pr: e.matmul(
                                        sml[:, 64 + pr * 16:64 + (pr + 1) * 16], wac[:, c, 512 + pr * 128:512 + (pr + 1) * 128],
                                        xhT[:, c, :], start=(c == 0), stop=(c == 7), skip_group_check=True),
                                        reads=[b_xh, b_w], writes=[b_sml], mark=(c == 7))
                            ctx.op("dve", lambda e: e.tensor_copy(cc[:, :, 0:16], sml[:, 64:96].rearrange("p (a t) -> p a t", a=2)),
                                   reads=[b_sml], writes=[b_cT[ci]])
                            yield
                        else:
                            pc = cT[1 - ci]
                            ctx.op("pool", lambda e: e.tensor_copy(cc[:, :, 0:16], pc[:, :, 512:528]),
                                   reads=[b_cT[1 - ci]], writes=[b_cT[ci]])
                            yield
                        for pr in range(2):
                            mi = pr
                            for c in range(8):
                                ctx.op("pe", lambda e, c=c, pr=pr: e.matmul(
                                    mm[mi][:], wac[:, c, 512 + pr * 128:512 + (pr + 1) * 128], xT[:, c, :],
                                    start=(c == 0), stop=(c == 7)), reads=[b_xT, b_w], writes=[b_mm[mi]], mark=(c == 7))
                            ctx.op("act", lambda e, pr=pr: e.copy(cc[:, pr, 16:528], mm[mi][:]), reads=[b_mm[mi]], writes=[b_cT[ci]])
                            yield
                        Rp = [b_cT[ci], b_sA, b_sB, b_w]
                        ctx.op("pool", lambda e: e.tensor_tensor(sA[:, :, 1:528], cc[:, :, 1:528], cc[:, :, 0:527], ALU.add),
                               reads=Rp, writes=[b_sA])
                        yield
                        ctx.op("pool", lambda e: e.tensor_tensor(sB[:, :, 3:528], sA[:, :, 3:528], sA[:, :, 1:526], ALU.add),
                               reads=Rp, writes=[b_sB])
                        yield
                        if gg == 0:
                            ctx.op("dve", lambda e: e.tensor_tensor(sA[0:64, 0, 16:32], sA[0:64, 0, 16:32], corr[0:64, 0, :], ALU.mult),
                                   reads=Rp, writes=[b_sA])
                            yield
                        ctx.op("dve", lambda e: e.scalar_tensor_tensor(ymT[0:64, 0, :], sA[0:64, 0, 16:528], 0.5, cc[0:64, 0, 16:528],
                                                                       ALU.mult, ALU.subtract), reads=Rp + [b_ymT], writes=[b_ymT])
                        yield
                        ctx.op("pool", lambda e: e.tensor_tensor(sA[:, :, 7:528], sB[:, :, 7:528], sB[:, :, 3:524], ALU.add),
                               reads=Rp + [b_ymT], writes=[b_sA])
                        yield
                        if gg == 0:
                            ctx.op("dve", lambda e: e.tensor_tensor(sB[64:128, 0, 16:32], sB[64:128, 0, 16:32], corr[64:128, 0, :], ALU.mult),
                                   reads=Rp, writes=[b_sB])
                            yield
                        ctx.op("dve", lambda e: e.scalar_tensor_tensor(ymT[64:128, 0, :], sB[64:128, 0, 16:528], 0.25, cc[64:128, 0, 16:528],
                                                                       ALU.mult, ALU.subtract), reads=Rp + [b_ymT], writes=[b_ymT])
                        yield
                        ctx.op("pool", lambda e: e.tensor_tensor(sB[:, :, 15:528], sA[:, :, 15:528], sA[:, :, 7:520], ALU.add),
                               reads=Rp + [b_ymT], writes=[b_sB])
                        yield
                        if gg == 0:
                            ctx.op("dve", lambda e: e.tensor_tensor(sA[0:64, 1, 16:32], sA[0:64, 1, 16:32], corr[0:64, 1, :], ALU.mult),
                                   reads=Rp, writes=[b_sA])
                            yield
                            ctx.op("dve", lambda e: e.tensor_tensor(sB[64:128, 1, 16:32], sB[64:128, 1, 16:32], corr[64:128, 1, :], ALU.mult),
                                   reads=Rp, writes=[b_sB])
                            yield
                        ctx.op("dve", lambda e: e.scalar_tensor_tensor(ymT[0:64, 1, :], sA[0:64, 1, 16:528], 0.125, cc[0:64, 1, 16:528],
                                                                       ALU.mult, ALU.subtract), reads=Rp + [b_ymT], writes=[b_ymT])
                        yield
                        ctx.op("dve", lambda e: e.scalar_tensor_tensor(ymT[64:128, 1, :], sB[64:128, 1, 16:528], 0.0625, cc[64:128, 1, 16:528],
                                                                       ALU.mult, ALU.subtract), reads=Rp + [b_ymT], writes=[b_ymT])
                        yield
                        for pr in range(2):
                            mi = pr
                            ctx.op("pe", lambda e, pr=pr: e.matmul(mm[mi][:], bdb[:, pr, :], ymT[:, pr, :], start=True, stop=True),
                                   reads=[b_ymT, b_w], writes=[b_mm[mi]])
                            ctx.op("act", lambda e, pr=pr: e.copy(mixT[:, 6 + pr, :], mm[mi][:]), reads=[b_mm[mi]], writes=[b_mixT])
                            yield
                        pool_done[0] = True

                    def tile_gen(s):
                        tl = gg * 4 + s
                        tb_i = tl - blk * TPB
                        _T = TSETS[tl % 2]
                        z, gst, gmv, gsm, vn, ya, xm, xmid = (_T[k_] for k_ in ("z", "gst", "gmv", "gsm", "vn", "ya", "xm", "xmid"))
                        lnt, lst, lmv, lsm, xlT, rt = (_T[k_] for k_ in ("lnt", "lst", "lmv", "lsm", "xlT", "rt"))
                        b_z, b_g, b_xm, b_xmid, b_ln, b_xmTf, b_rt = (_T[k_] for k_ in ("b_z", "b_g", "b_xm", "b_xmid", "b_ln", "b_xmTf", "b_rt"))
                        xi = tl % 4
                        oi = tl % 2
                        ctx.dma("sp", ob[oi][:], P["o"][tl * 128:(tl + 1) * 128, :], reads=([P["o_rd"][tl]] if "o_rd" in P else []), writes=[b_ob[oi]])
                        yield
                        mi_ = tl % 2
                        for c in range(8):
                            ctx.op("pe", lambda e, c=c: e.matmul(mm[mi_][:], xT[:, c, s * 128:(s + 1) * 128], wac[:, c, 0:512],
                                                                 start=(c == 0), stop=(c == 7)),
                                   reads=[b_xT, b_w], writes=[b_mm[mi_]], mark=(c == 7))
                        ctx.op("act", lambda e: e.activation(z[:], mm[mi_][:], AF.Gelu), reads=[b_mm[mi_]], writes=[b_z])
                        yield
                        Rg = [b_z, b_g, b_w]
                        for h in range(4):
                            ctx.op("dve", lambda e, h=h: e.bn_stats(gst[:, h, :], z[:, 256 + h * 64:256 + (h + 1) * 64]),
                                   reads=Rg, writes=[b_g])
                            yield
                        for h in range(4):
                            ctx.op("dve", lambda e, h=h: e.bn_aggr(gmv[:, h, :], gst[:, h, :]), reads=Rg, writes=[b_g])
                            yield
                        ctx.op("dve", lambda e: e.tensor_scalar(gsm[:, 0:4], gmv[:, :, 1], EPS, None, ALU.add), reads=Rg, writes=[b_g])
                        yield
                        for h in range(4):
                            ctx.op("pool", lambda e, h=h: e.tensor_tensor(gsm[:, 4 + h:5 + h], gsm[:, h:h + 1], neghalf[:], ALU.pow),
                                   reads=Rg + [b_const], writes=[b_g])
                            yield
                        for h in range(4):
                            ctx.op("dve", lambda e, h=h: e.tensor_scalar(
                                vn[:, h * 64:(h + 1) * 64], z[:, 256 + h * 64:256 + (h + 1) * 64], gmv[:, h, 0:1], gsm[:, 4 + h:5 + h],
                                ALU.subtract, ALU.mult), reads=Rg, writes=[b_g])
                            yield
                        for h in range(4):
                            ctx.op("pe", lambda e, h=h: e.matmul(sml[:, mi_ * 256 + h * 64:mi_ * 256 + (h + 1) * 64], wmTb[:, h, :], vn[:, h * 64:(h + 1) * 64],
                                                                 start=True, stop=True, skip_group_check=True),
                                   reads=[b_g, b_w], writes=[b_sml], mark=(h == 3))
                        for h in range(4):
                            ctx.op("dve", lambda e, h=h: e.scalar_tensor_tensor(
                                ya[:, h * 64:(h + 1) * 64], sml[:, mi_ * 256 + h * 64:mi_ * 256 + (h + 1) * 64], gbs[:, h:h + 1], z[:, h * 64:(h + 1) * 64],
                                ALU.add, ALU.mult), reads=Rg + [b_sml], writes=[b_g])
                            yield
                        for k2 in range(2):
                            ctx.op("pe", lambda e, k2=k2: e.transpose(tpb[:, k2 * 128:(k2 + 1) * 128], ya[:, k2 * 128:(k2 + 1) * 128],
                                                                      identb[:]), reads=[b_g, b_const], writes=[b_tpb], mark=False)
                        for k4 in range(4):
                            ctx.op("pe", lambda e, k4=k4: e.transpose(tpb[:, (2 + k4) * 128:(3 + k4) * 128],
                                                                      ob[oi][:, k4 * 128:(k4 + 1) * 128], identb[:]),
                                   reads=[b_ob[oi], b_const], writes=[b_tpb], mark=(k4 == 3))
                        ctx.op("dve", lambda e: e.tensor_copy(mixT[:, 0:6, s * 128:(s + 1) * 128],
                                                              tpb[:, 0:768].rearrange("p (c t) -> p c t", c=6)),
                               reads=[b_tpb], writes=[b_mixT])
                        yield
                        while not pool_done[0]:
                            yield
                        for hf in range(2):
                            for c in range(8):
                                ctx.op("pe", lambda e, c=c, hf=hf: e.matmul(hh[hf][:], mixT[:, c, s * 128:(s + 1) * 128],
                                                                            wout[:, c, hf * 512:(hf + 1) * 512],
                                                                            start=(c == 0), stop=(c == 7)),
                                       reads=[b_mixT, b_w], writes=[b_hh[hf]], mark=(c == 7))
                        for hf in range(2):
                            ctx.op("dve", lambda e, hf=hf: e.scalar_tensor_tensor(
                                xm[:, hf * 512:(hf + 1) * 512], xf[xi][:, hf * 512:(hf + 1) * 512], float(ALPHA), hh[hf][:],
                                ALU.mult, ALU.add), reads=[b_xf[xi], b_hh[hf], b_ln], writes=[b_xm])
                        yield
                        Rl = [b_xm, b_ln, b_w, b_const, b_xmid]
                        yield from _ln_tile(ctx, xm, xmid, lng, lnb, lst, lmv, lsm, lnt, Rl, [b_ln, b_xmid], neghalf)
                        if "dbg_z" in P and tl == 0:
                            for nm, t_, bb in (("dbg_z", z, b_z), ("dbg_mix", mixT, b_mixT), ("dbg_xm", xm, b_xm),
                                               ("dbg_xmid", xmid, b_xmid), ("dbg_ya", ya, b_g), ("dbg_xT", xT, b_xT)):
                                ob_ = Buf(); ctx.outbufs.append(ob_)
                                ctx.dma("sp", P[nm], t_[:], reads=[bb], writes=[ob_])
                                yield
                        ctx.op("act", lambda e: e.activation(yacc[:, tb_i, :], xmid[:], AF.Copy, scale=float(ALPHA)),
                               reads=[b_xmid], writes=[b_yacc[tb_i]])
                        yield
                        for q4 in range(2):
                            ti = q4
                            for c4 in range(4):
                                c = q4 * 4 + c4
                                ctx.op("pe", lambda e, c=c, c4=c4: e.transpose(
                                    tp[ti][:, c4 * 128:(c4 + 1) * 128], xmid[:, c * 128:(c + 1) * 128], identf[:]),
                                    reads=[b_xmid, b_const], writes=[b_tp[ti]], mark=(c4 == 3))
                            src = tp[ti][:].rearrange("p (c t) -> p c t", c=4)
                            ctx.op("act", lambda e: e.copy(xmT[:, q4 * 4:(q4 + 1) * 4, tb_i * 128:(tb_i + 1) * 128], src),
                                   reads=[b_tp[ti]], writes=[b_xmT[tb_i]])
                            if moe:
                                ctx.op("dve", lambda e: e.tensor_tensor(
                                    xlT[:, q4 * 4:(q4 + 1) * 4, :], src,
                                    xmT[:, q4 * 4:(q4 + 1) * 4, tb_i * 128:(tb_i + 1) * 128], ALU.subtract),
                                    reads=[b_tp[ti], b_xmT[tb_i]], writes=[b_xmTf])
                                yield
                        if moe:
                            terms = [(xmT, rwh, True), (xmT, rwl, True), (xlT, rwh, False)]
                            nmm = 0
                            for (xa, wb, is_hi) in terms:
                                for c in range(8):
                                    lhs = xa[:, c, tb_i * 128:(tb_i + 1) * 128] if is_hi else xa[:, c, :]
                                    ctx.op("pe", lambda e, c=c, lhs=lhs, wb=wb, nmm=nmm: e.matmul(
                                        mm[mi_][:, 0:8], lhs, wb[:, c, :], start=(nmm == 0), stop=(nmm == 23),
                                        skip_group_check=True),
                                        reads=[b_xmTf, b_xmT[tb_i], b_w], writes=[b_mm[mi_]], mark=(nmm == 23))
                                    nmm += 1
                            Rr = [b_rt, b_mm[mi_]]
                            Wr = [b_rt]
                            lg, eq1, l2, eq2 = rt[:, 0:8], rt[:, 8:16], rt[:, 16:24], rt[:, 24:32]
                            m1, m2, dl, ex, w1, w2 = (rt[:, 32 + i:33 + i] for i in range(6))
                            g1 = rt[:, 40:48]
                            ctx.op("dve", lambda e: e.tensor_copy(lg, mm[mi_][:, 0:8]), reads=Rr, writes=Wr)
                            yield
                            if RDBG == 1:
                                ctx.op("dve", lambda e: e.tensor_copy(gates[:, tb_i, :], lg), reads=Rr, writes=Wr + [b_gates[tb_i]])
                                yield
                                return
                            ctx.op("dve", lambda e: e.tensor_reduce(m1, lg, AX.X, ALU.max), reads=Rr, writes=Wr)
                            yield
                            ctx.op("dve", lambda e: e.tensor_scalar(eq1, lg, m1, None, ALU.is_equal), reads=Rr, writes=Wr)
                            yield
                            ctx.op("dve", lambda e: e.scalar_tensor_tensor(l2, eq1, -1e30, lg, ALU.mult, ALU.add), reads=Rr, writes=Wr)
                            yield
                            ctx.op("dve", lambda e: e.tensor_reduce(m2, l2, AX.X, ALU.max), reads=Rr, writes=Wr)
                            yield
                            ctx.op("dve", lambda e: e.tensor_scalar(eq2, l2, m2, None, ALU.is_equal), reads=Rr, writes=Wr)
                            yield
                            ctx.op("dve", lambda e: e.tensor_tensor(dl, m2, m1, ALU.subtract), reads=Rr, writes=Wr)
                            yield
                            ctx.op("act", lambda e: e.activation(ex, dl, AF.Exp), reads=Rr, writes=Wr)
                            yield
                            ctx.op("dve", lambda e: e.tensor_scalar(w1, ex, 1.0, None, ALU.add), reads=Rr, writes=Wr)
                            yield
                            ctx.op("dve", lambda e: e.reciprocal(w1, w1), reads=Rr, writes=Wr)
                            yield
                            ctx.op("dve", lambda e: e.tensor_tensor(w2, ex, w1, ALU.mult), reads=Rr, writes=Wr)
                            yield
                            ctx.op("dve", lambda e: e.tensor_scalar(g1, eq1, w1, None, ALU.mult), reads=Rr, writes=Wr)
                            yield
                            ctx.op("dve", lambda e: e.scalar_tensor_tensor(gates[:, tb_i, :], eq2, w2, g1, ALU.mult, ALU.add),
                                   reads=Rr, writes=Wr + [b_gates[tb_i]])
                            yield
                    _live = [(0, tile_gen(0)), (1, tile_gen(1)), (-1, pool_gen())]
                    while _live:
                        for _it in list(_live):
                            try:
                                next(_it[1])
                            except StopIteration:
                                _live.remove(_it)
                                if 0 <= _it[0] and _it[0] + 2 < 4:
                                    _live.append((_it[0] + 2, tile_gen(_it[0] + 2)))
            ctx.barrier()

            with contextlib.ExitStack() as es2:
                w1s = [_sb(es2, nc, "f_w1_%d" % i, [128, 8, 512], BF16) for i in range(2)]
                w3s = [_sb(es2, nc, "f_w3_%d" % i, [128, 8, 512], BF16) for i in range(2)]
                w2s = [_sb(es2, nc, "f_w2_%d" % i, [128, 4, 1024], BF16) for i in range(2)]
                gT = [_sb(es2, nc, "f_gT%d" % i, [128, 4, 512], BF16) for i in range(2)]
                sl = [_sb(es2, nc, "f_sl%d" % i, [128, 512], F32) for i in range(2)]
                lng = _sb(es2, nc, "f_lng", [128, 1024], F32)
                lnb = _sb(es2, nc, "f_lnb", [128, 1024], F32)
                lnt = _sb(es2, nc, "f_lnt", [128, 1024], F32)
                xo = [_sb(es2, nc, "f_xo%d" % i, [128, 1024], F32) for i in range(2)]
                lst = _sb(es2, nc, "f_lst", [128, 2, 6], F32)
                lmv = _sb(es2, nc, "f_lmv", [128, 2], F32)
                lsm = _sb(es2, nc, "f_lsm", [128, 4], F32)
                h1p = [_ps(es2, nc, "f_h1_%d" % i, [128, 512], F32) for i in range(2)]
                h3p = [_ps(es2, nc, "f_h3_%d" % i, [128, 512], F32) for i in range(2)]
                yp = [_ps(es2, nc, "f_y_%d" % i, [128, 512], F32) for i in range(2)]
                b_wg = [Buf(), Buf()]
                b_gT = [Buf(), Buf()]
                b_sl = [Buf(), Buf()]
                b_h1 = [Buf(), Buf()]
                b_h3 = [Buf(), Buf()]
                b_yp = [Buf(), Buf()]
                b_lnw, b_ln = Buf(), Buf()
                b_xo = [Buf(), Buf()]
                ctx.dma("sp", lng[:], P["ln2g"][:, :], writes=[b_lnw])
                ctx.dma("sp", lnb[:], P["ln2b"][:, :], writes=[b_lnw])
                if "xg_in" in P:
                    xo4 = [_sb(es2, nc, "f_xo4_%d" % i, [128, 4, 1024], BF16) for i in range(2)]
                    b_xo4 = [Buf(), Buf()]
                    maskt = _sb(es2, nc, "f_mask", [128, 4], F32)
                    ctx.dma("sp", maskt[:], P["mask"][:, :], writes=[b_lnw])

                work = [(e_, f0_, gsz_) for e_ in range(nexp) for (f0_, gsz_) in groups]

                def load_w(n):
                    e_, f0_, gsz_ = work[n]
                    wi = n % 2
                    c0, c1 = f0_ * 128, (f0_ + gsz_) * 128
                    if moe:
                        s1, s3, s2 = P["w1"][e_], P["w3"][e_], P["w2"][e_]
                    else:
                        s1, s3, s2 = P["w1"], P["w3"], P["w2"]
                    ctx.dma("pool", w1s[wi][:, :, 0:gsz_ * 128], s1[:, c0:c1].rearrange("(c p) n -> p c n", p=128),
                            writes=[b_wg[wi]])
                    ctx.dma("pool", w3s[wi][:, :, 0:gsz_ * 128], s3[:, c0:c1].rearrange("(c p) n -> p c n", p=128),
                            writes=[b_wg[wi]])
                    ctx.dma("pool", w2s[wi][:, 0:gsz_, :], s2[c0:c1, :].rearrange("(c p) n -> p c n", p=128),
                            writes=[b_wg[wi]])

                load_w(0)
                hcnt = 0
                ycnt = 0
                gcnt = 0
                for n in range(len(work)):
                    if n + 1 < len(work):
                        load_w(n + 1)
                    e_, f0_, gsz_ = work[n]
                    wi = n % 2
                    for t5 in range(GPB):
                        gi = gcnt % 2
                        gcnt += 1
                        tiles5 = [b_xmT[t5 * 4 + k] for k in range(4)]
                        for fc in range(gsz_):
                            hi = hcnt % 2
                            hcnt += 1
                            for c in range(8):
                                ctx.op("pe", lambda e, c=c: e.matmul(h1p[hi][:], w1s[wi][:, c, fc * 128:(fc + 1) * 128],
                                                                     xmT[:, c, t5 * 512:(t5 + 1) * 512], start=(c == 0), stop=(c == 7)),
                                       reads=[b_wg[wi]] + tiles5, writes=[b_h1[hi]], mark=(c == 7))
                            for c in range(8):
                                ctx.op("pe", lambda e, c=c: e.matmul(h3p[hi][:], w3s[wi][:, c, fc * 128:(fc + 1) * 128],
                                                                     xmT[:, c, t5 * 512:(t5 + 1) * 512], start=(c == 0), stop=(c == 7)),
                                       reads=[b_wg[wi]] + tiles5, writes=[b_h3[hi]], mark=(c == 7))
                            ctx.op("act", lambda e: e.activation(sl[hi][:], h1p[hi][:], AF.Silu), reads=[b_h1[hi]], writes=[b_sl[hi]])
                            ctx.op("dve", lambda e: e.tensor_tensor(gT[gi][:, fc, :], sl[hi][:], h3p[hi][:], ALU.mult),
                                   reads=[b_sl[hi], b_h3[hi]], writes=[b_gT[gi]])
                        for k in range(4):
                            tb_i = t5 * 4 + k
                            for hf in range(2):
                                yi = ycnt % 2
                                ycnt += 1
                                for fc in range(gsz_):
                                    ctx.op("pe", lambda e, fc=fc: e.matmul(yp[yi][:], gT[gi][:, fc, k * 128:(k + 1) * 128],
                                                                           w2s[wi][:, fc, hf * 512:(hf + 1) * 512],
                                                                           start=(fc == 0), stop=(fc == gsz_ - 1)),
                                           reads=[b_gT[gi], b_wg[wi]], writes=[b_yp[yi]], mark=(fc == gsz_ - 1))
                                sc = gates[:, tb_i, e_:e_ + 1] if moe else 1.0
                                rd = [b_yp[yi], b_yacc[tb_i]] + ([b_gates[tb_i]] if moe else [])
                                ctx.op("dve", lambda e: e.scalar_tensor_tensor(
                                    yacc[:, tb_i, hf * 512:(hf + 1) * 512], yp[yi][:], sc, yacc[:, tb_i, hf * 512:(hf + 1) * 512],
                                    ALU.mult, ALU.add), reads=rd, writes=[b_yacc[tb_i]])
                for tb_i in range(TPB):
                    tl = blk * TPB + tb_i
                    oi = tb_i % 2
                    Rl = [b_yacc[tb_i], b_ln, b_lnw, b_const, b_xo[oi]]
                    for _ in _ln_tile(ctx, yacc[:, tb_i, :], xo[oi], lng, lnb, lst, lmv, lsm, lnt, Rl, [b_ln, b_xo[oi]], neghalf):
                        pass
                    if "xg_in" not in P:
                        ob_ = Buf()
                        ctx.outbufs.append(ob_)
                        ctx.dma("sp", P["xo"][tl * 128:(tl + 1) * 128, :], xo[oi][:], reads=[b_xo[oi]], writes=[ob_])
                    else:
                        ctx.dma("sp", P["xo"][tl * 128:(tl + 1) * 128, :], xo[oi][:], reads=[b_xo[oi]], writes=[P["xo_buf"]])
                        x4 = xo4[oi]
                        for sl_ in range(4):
                            eng_ = "act" if sl_ % 2 == 0 else "dve"
                            if eng_ == "act":
                                ctx.op("act", lambda e, sl_=sl_: e.activation(x4[:, sl_, :], xo[oi][:], AF.Copy,
                                                                              scale=maskt[:, sl_:sl_ + 1]),
                                       reads=[b_xo[oi], b_lnw, b_xo4[oi]], writes=[b_xo4[oi]])
                            else:
                                ctx.op("dve", lambda e, sl_=sl_: e.tensor_scalar(x4[:, sl_, :], xo[oi][:], maskt[:, sl_:sl_ + 1],
                                                                                None, ALU.mult),
                                       reads=[b_xo[oi], b_lnw, b_xo4[oi]], writes=[b_xo4[oi]])
                        xt_, xbuf_ = P["xg_in"][tl // 4]
                        r0 = (tl % 4) * 128
                        ctx.dma("sp", xt_.ap().rearrange("(j t) d -> t j d", j=4)[r0:r0 + 128, :, :], x4[:],
                                reads=[b_xo4[oi]], writes=[xbuf_])
                        if "on_chunk" in P and tl % 4 == 3:
                            P["on_chunk"](tl // 4)
            ctx.barrier()


def _tok_consts(r_is_first):
    maskT = (np.arange(128)[:, None] <= np.arange(128)[None, :]).astype(np.float32)
    corr = np.ones((128, 2, 16), np.float32)
    if r_is_first:
        wins = (2, 4, 8, 16)
        t = np.arange(16)
        for g in range(4):
            w = wins[g]
            corr[(g % 2) * 64:(g % 2 + 1) * 64, g // 2, :] = (w / np.minimum(t + 1, w)).astype(np.float32)[None, :]
    return maskT, corr


def _rep(v, n=128):
    return np.ascontiguousarray(np.broadcast_to(np.asarray(v, np.float32)[None, :], (n, v.shape[0])), dtype=np.float32)


def _tok_inputs(inp, l, x_shard, x_halo, o_shard, r_is_first):
    moe = (l % 2 == 1)
    w_in = inp["w_in"][l]
    maskT, corr = _tok_consts(r_is_first)
    m = {}
    if x_shard is not None:
        m.update({"x": np.ascontiguousarray(x_shard, dtype=np.float32),
                  "xh": np.ascontiguousarray(x_halo, dtype=np.float32),
                  "o": np.ascontiguousarray(o_shard)})
    m.update({
        "wac": np.ascontiguousarray(np.concatenate([w_in[:, 0:O_A], w_in[:, O_V:D_IN]], axis=1), dtype=np.float32),
        "wout": np.ascontiguousarray(inp["w_out"][l], dtype=np.float32),
        "gws": np.ascontiguousarray(np.transpose(inp["gmlp_ws"][l], (0, 2, 1)), dtype=np.float32),
        "maskT": maskT,
        "gbs": np.ascontiguousarray(inp["gmlp_bs"][l].T, dtype=np.float32),
        "poolw": np.ascontiguousarray(inp["pool_w"][l], dtype=np.float32),
        "psc": _rep(inp["pool_scale"][l]),
        "corr": corr,
        "ln1g": _rep(inp["ln1_g"][l]), "ln1b": _rep(inp["ln1_b"][l]),
        "ln2g": _rep(inp["ln2_g"][l]), "ln2b": _rep(inp["ln2_b"][l]),
        "eye": np.eye(128, dtype=np.float32),
    })
    i = l // 2
    if moe:
        m["rw"] = np.ascontiguousarray(inp["router_w"][i], dtype=np.float32)
        m["w1"] = inp["moe_w1"][i]
        m["w3"] = inp["moe_w3"][i]
        m["w2"] = inp["moe_w2"][i]
    else:
        m["w1"] = inp["ffn_w1"][i]
        m["w3"] = inp["ffn_w3"][i]
        m["w2"] = inp["ffn_w2"][i]
    return m


def _tok_dram(nc, moe, ntok, dbg=False, sfx="", fused=False):
    def di(name, shape, dt=F32):
        return nc.dram_tensor(name + sfx, list(shape), dt, kind="ExternalInput").ap()
    P = {}
    if not fused:
        P.update({"x": di("x", [ntok, D]), "xh": di("xh", [16, D]), "o": di("o", [ntok, 512], BF16)})
    P.update({
        "wac": di("wac", [D, 768]), "wout": di("wout", [D, D]), "gws": di("gws", [4, 128, 128]),
        "maskT": di("maskT", [128, 128]), "gbs": di("gbs", [128, 4]), "poolw": di("poolw", [4, 64, 64]),
        "psc": di("psc", [128, 256]), "corr": di("corr", [128, 2, 16]),
        "ln1g": di("ln1g", [128, D]), "ln1b": di("ln1b", [128, D]), "ln2g": di("ln2g", [128, D]), "ln2b": di("ln2b", [128, D]),
        "eye": di("eye", [128, 128]),
    })
    if moe:
        P["rw"] = di("rw", [D, NE])
        P["w1"] = di("w1", [NEC, D, D_FFE])
        P["w3"] = di("w3", [NEC, D, D_FFE])
        P["w2"] = di("w2", [NEC, D_FFE, D])
    else:
        P["w1"] = di("w1", [D, D_FF])
        P["w3"] = di("w3", [D, D_FF])
        P["w2"] = di("w2", [D_FF, D])
    if not fused:
        P["xo"] = nc.dram_tensor("xo", [ntok, D], F32, kind="ExternalOutput").ap()
    if dbg:
        def do(name, shape, dt=F32):
            return nc.dram_tensor(name, list(shape), dt, kind="ExternalOutput").ap()
        P["dbg_z"] = do("dbg_z", [128, 512]); P["dbg_mix"] = do("dbg_mix", [128, 8, 512], BF16)
        P["dbg_xm"] = do("dbg_xm", [128, 1024]); P["dbg_xmid"] = do("dbg_xmid", [128, 1024])
        P["dbg_ya"] = do("dbg_ya", [128, 256], BF16); P["dbg_xT"] = do("dbg_xT", [128, 8, 512], BF16)
    return P


def build_tok_program(l, ntok=TSH, tb=1024, dbg=False):
    moe = (l % 2 == 1)
    nc = bass.Bass("TRN2", target_bir_lowering=False)
    P = _tok_dram(nc, moe, ntok, dbg)
    with contextlib.ExitStack() as es:
        ctx = Ctx(nc, es)
        tok_phase(ctx, P, moe, ntok=ntok, tb=tb)
        ctx.finish(ctx.outbufs)
    return nc


_PROG_CACHE = {}


def _get_prog(kind, l):
    key = (kind, l)
    if key not in _PROG_CACHE:
        _PROG_CACHE[key] = build_attn_program(l) if kind == "attn" else build_tok_program(l)
    return _PROG_CACHE[key]


def kernel_unfused(**inputs):
    inp = {k: np.asarray(v) for k, v in inputs.items()}
    x = np.ascontiguousarray(inp["x"], dtype=np.float32)
    cores = list(range(NCORES))
    nsh = S // TSH
    for l in range(DEPTH):
        nc = _get_prog("attn", l)
        in_maps = []
        for c in cores:
            b, h = c // NH, c % NH
            m = _attn_inputs(inp, l, b, h)
            m["x"] = x[b]
            in_maps.append(m)
        res = run_bass_kernel_spmd(nc, in_maps, core_ids=cores)
        o_full = [np.concatenate([np.asarray(res.results[b * NH + h]["o"]) for h in range(NH)], axis=1) for b in range(B)]
        nc = _get_prog("tok", l)
        in_maps = []
        for c in cores:
            b, r = c // nsh, c % nsh
            xs = x[b, r * TSH:(r + 1) * TSH]
            xh = x[b, r * TSH - 16:r * TSH] if r > 0 else np.zeros((16, D), np.float32)
            in_maps.append(_tok_inputs(inp, l, xs, xh, o_full[b][r * TSH:(r + 1) * TSH], r == 0))
        res = run_bass_kernel_spmd(nc, in_maps, core_ids=cores)
        x = np.stack([np.concatenate([np.asarray(res.results[b * nsh + r]["xo"]) for r in range(nsh)], axis=0)
                      for b in range(B)]).astype(np.float32)
    return x


def kernel(**inputs):
    inp = {k: np.asarray(v) for k, v in inputs.items()}
    inp["x"] = np.ascontiguousarray(inp["x"], dtype=np.float32)
    if "fused" not in _PROG_CACHE:
        _PROG_CACHE["fused"] = build_fused_program()
    nc = _PROG_CACHE["fused"]
    cores = list(range(NCORES))
    in_maps = [_fused_inputs(inp, c) for c in cores]
    res = run_bass_kernel_spmd(nc, in_maps, core_ids=cores)
    nsh = S // TSH
    return np.stack([np.concatenate([np.asarray(res.results[b * nsh + r]["xo"]) for r in range(nsh)], axis=0)
                     for b in range(B)]).astype(np.float32)


GROUPS = [[0, 1, 2, 3], [4, 5, 6, 7]]


def o_combine_alloc(es, nc):
    NS = 4
    return dict(
        NS=NS,
        maskt=_sb(es, nc, "c_mask", [128, 4], F32),
        ld=[[_sb(es, nc, "c_ld%d_%d" % (i, k), [128, 512], BF16) for k in range(4)] for i in range(NS)],
        acc=[_sb(es, nc, "c_acc%d" % i, [128, 512], F32) for i in range(2)],
        res=[_sb(es, nc, "c_res%d" % i, [128, 512], BF16) for i in range(NS)],
        b_m=Buf(), b_ld=[Buf() for _ in range(NS)], b_acc=[Buf(), Buf()], b_res=[Buf() for _ in range(NS)], loaded=[False])


def o_combine(ctx, C, og_out, mask_d, o_mine_t, o_bufs):
    NS = C["NS"]
    maskt, ld, acc, res = C["maskt"], C["ld"], C["acc"], C["res"]
    b_m, b_ld, b_acc, b_res = C["b_m"], C["b_ld"], C["b_acc"], C["b_res"]
    if not C["loaded"][0]:
        ctx.dma("sp", maskt[:], mask_d[:, :], writes=[b_m])
        C["loaded"][0] = True
    for tl in range(TSH // 128):
        i = tl % NS
        a2 = tl % 2
        for k in range(4):
            t_, buf_ = og_out[k]
            ctx.dma("sp", ld[i][k][:].rearrange("p (j d) -> p j d", j=4),
                    t_.ap().rearrange("(j t) d -> t j d", j=4)[tl * 128:(tl + 1) * 128, :, :],
                    reads=[buf_], writes=[b_ld[i]])
        ctx.op("dve", lambda e: e.tensor_scalar(acc[a2][:], ld[i][0][:], maskt[:, 0:1], None, ALU.mult),
               reads=[b_ld[i], b_m, b_acc[a2]], writes=[b_acc[a2]])
        for k in range(1, 4):
            dst = res[i] if k == 3 else acc[a2]
            ctx.op("dve", lambda e, k=k, dst=dst: e.scalar_tensor_tensor(dst[:], ld[i][k][:], maskt[:, k:k + 1], acc[a2][:],
                                                                       ALU.mult, ALU.add),
                   reads=[b_ld[i], b_m, b_acc[a2], b_res[i]], writes=[b_res[i] if k == 3 else b_acc[a2]])
        ctx.dma("act", o_mine_t.ap()[tl * 128:(tl + 1) * 128, :], res[i][:], reads=[b_res[i]], writes=[o_bufs[tl]])


def halo_combine(ctx, xg7, mask_d, xh_t, xh_buf):
    nc = ctx.nc
    t_, buf_ = xg7
    with contextlib.ExitStack() as es:
        maskt = _sb(es, nc, "h_mask", [128, 4], F32)
        t3 = _sb(es, nc, "h_t3", [16, 3, 1024], BF16)
        acc = _sb(es, nc, "h_acc", [16, 1024], F32)
        b_m, b_t, b_a = Buf(), Buf(), Buf()
        ctx.dma("sp", maskt[:], mask_d[:, :], writes=[b_m])
        for j in range(1, 4):
            r0 = (j - 1) * 512 + 496
            ctx.dma("sp", t3[:, j - 1, :], t_.ap()[r0:r0 + 16, :], reads=[buf_], writes=[b_t])
        ctx.op("dve", lambda e: e.tensor_scalar(acc[:], t3[:, 0, :], maskt[0:16, 1:2], None, ALU.mult),
               reads=[b_t, b_m], writes=[b_a])
        for j in (2, 3):
            ctx.op("dve", lambda e, j=j: e.scalar_tensor_tensor(acc[:], t3[:, j - 1, :], maskt[0:16, j:j + 1], acc[:],
                                                               ALU.mult, ALU.add), reads=[b_t, b_m, b_a], writes=[b_a])
        ctx.dma("sp", xh_t.ap()[:, :], acc[:], reads=[b_a], writes=[xh_buf])
    ctx.barrier()


def build_fused_program():
    nc = bass.Bass("TRN2", target_bir_lowering=False)

    def di(name, shape, dt=F32):
        return nc.dram_tensor(name, list(shape), dt, kind="ExternalInput").ap()
    xfull = di("xfull", [S, D])
    A = []
    for l in range(DEPTH):
        A.append({"wqkv": di("wqkv_%d" % l, [D, 384]), "lam": di("lam_%d" % l, [128, 256]), "subg": di("subg_%d" % l, [128, 128])})
    bd_d, bn_d, bfar_d = di("bd", [128, 128]), di("bn", [128, 128]), di("bfar", [128, 1])
    eye_d, mask_d = di("eye_a", [128, 128]), di("mask", [128, 4])
    P = [_tok_dram(nc, (l % 2 == 1), TSH, sfx="_%d" % l, fused=True) for l in range(DEPTH)]
    xs_d, xh_d = di("xs", [TSH, D]), di("xh", [16, D])
    xout = nc.dram_tensor("xo", [TSH, D], F32, kind="ExternalOutput").ap()
    og_in = [[(nc.dram_tensor("og_in_%d_%d" % (l, k), [4 * TSH, 128], BF16), Buf()) for k in range(4)] for l in range(DEPTH)]
    og_out = [[(nc.dram_tensor("og_out_%d_%d" % (l, k), [4 * TSH, 128], BF16), Buf()) for k in range(4)] for l in range(DEPTH)]
    xg_in = [(nc.dram_tensor("xg_in_%d" % k, [4 * 512, D], BF16), Buf()) for k in range(8)]
    xg_out = [(nc.dram_tensor("xg_out_%d" % k, [4 * 512, D], BF16), Buf()) for k in range(8)]
    o_mine = [nc.dram_tensor("o_mine_%d" % l, [TSH, 512], BF16) for l in range(DEPTH)]
    x1_loc = nc.dram_tensor("x1_loc", [TSH, D], F32)
    xh_loc = nc.dram_tensor("xh_loc", [16, D], F32)
    with contextlib.ExitStack() as es:
        ctx = Ctx(nc, es)
        x1_buf, xh_buf = Buf(), Buf()
        OC = o_combine_alloc(es, nc)
        for l in range(DEPTH):
            def _og_chunk(k, l=l):
                ctx.allreduce(og_in[l][k][0], og_out[l][k][0], GROUPS, reads=[og_in[l][k][1]], writes=[og_out[l][k][1]])
            attn_phase(ctx, xfull, A[l]["wqkv"], bd_d, bn_d, bfar_d, A[l]["lam"], A[l]["subg"], eye_d, None, _lam_init(l),
                       xg=(xg_out if l > 0 else None), og=og_in[l], mask_d=mask_d, on_chunk=_og_chunk)
            o_bufs = [Buf() for _ in range(TSH // 128)]
            o_combine(ctx, OC, og_out[l], mask_d, o_mine[l], o_bufs)
            Pl = P[l]
            Pl["o"] = o_mine[l].ap()
            Pl["o_rd"] = o_bufs
            Pl["mask"] = mask_d
            if l == 0:
                Pl["x"], Pl["xh"] = xs_d, xh_d
                Pl["xo"] = x1_loc.ap()
                Pl["xo_buf"] = x1_buf
                Pl["xg_in"] = xg_in

                def _xg_chunk(k):
                    ctx.allreduce(xg_in[k][0], xg_out[k][0], GROUPS, reads=[xg_in[k][1]], writes=[xg_out[k][1]])
                Pl["on_chunk"] = _xg_chunk
            else:
                Pl["x"], Pl["x_rd"] = x1_loc.ap(), [x1_buf]
                Pl["xh"], Pl["xh_rd"] = xh_loc.ap(), [xh_buf]
                Pl["xo"] = xout
            tok_phase(ctx, Pl, (l % 2 == 1))
            if l == 0:
                halo_combine(ctx, xg_out[7], mask_d, xh_loc, xh_buf)
        ctx.finish(ctx.outbufs)
    return nc


def _fused_inputs(inp, c):
    b, r = c // 4, c % 4
    x = inp["x"]
    m = {"xfull": x[b], "xs": np.ascontiguousarray(x[b, r * TSH:(r + 1) * TSH]),
         "xh": np.ascontiguousarray(x[b, r * TSH - 16:r * TSH]) if r > 0 else np.zeros((16, D), np.float32)}
    mask = np.zeros((128, 4), np.float32)
    mask[:, r] = 1.0
    m["mask"] = mask
    for l in range(DEPTH):
        a = _attn_inputs(inp, l, b, r)
        m["wqkv_%d" % l], m["lam_%d" % l], m["subg_%d" % l] = a["wqkv"], a["lam"], a["subg"]
        if l == 0:
            m["bd"], m["bn"], m["bfar"], m["eye_a"] = a["bd"], a["bn"], a["bfar"], a["eye"]
        t = _tok_inputs(inp, l, None, None, None, r == 0)
        for k, v in t.items():
            if k in ("x", "xh", "o"):
                continue
            m[k + "_%d" % l] = v
    return m
```
